# Optimizing a Trainium2 kernel written in Bass

```python
import math
import jax, jax.numpy as jnp
from jax import lax
import numpy as np

D_MODEL = 2048
BATCH = 8
SEQ = 4096
DEPTH = 4

GRID_W = 64
CTX_LEN = 256
N_MIXERS = 3
RMS_EPS = 1e-6

HG_HEADS = 16
HG_DK = D_MODEL // HG_HEADS
HG_DV = D_MODEL // HG_HEADS
HG_CHUNK = 64

S5_GROUP = 16
S5_GROUPS = D_MODEL // S5_GROUP
S5_STATE = 64
S5_CHUNK = 128
S5_DT_MIN = 1e-3
S5_DT_MAX = 1e-1

ATT_HEADS = 32
ATT_KV_HEADS = 8
ATT_HEAD_DIM = 64
ATT_WINDOW = 128
ATT_BLOCK = 128
ROPE_BASE = 10000.0

MOE_GROUPS = 4
MOE_EXPERTS_PER_GROUP = 8
MOE_EXPERTS = MOE_GROUPS * MOE_EXPERTS_PER_GROUP
MOE_TOP_K = 2
MOE_D_FF = 512

N_LAYERS_A = (DEPTH + 2) // 3
N_LAYERS_B = (DEPTH + 1) // 3
N_LAYERS_C = DEPTH // 3

kernel_name = "hybrid_hgrn2_s5_swa_hmoe_dit"


def _rmsnorm(x, g):
    x32 = x.astype(jnp.float32)
    y = x32 * lax.rsqrt(jnp.mean(x32 * x32, axis=-1, keepdims=True) + RMS_EPS)
    return (y * g.astype(jnp.float32)).astype(x.dtype)


def _modulate(h, shift, scale):
    return h * (1 + scale) + shift


def _hgrn_scan(q, k, v, g, s0, readout):
    bsz, length, heads, _ = q.shape
    n_chunks = length // HG_CHUNK

    def to_chunks(t):
        return t.reshape(bsz, n_chunks, HG_CHUNK, heads, t.shape[-1]).transpose(1, 0, 3, 2, 4)

    causal = jnp.tril(jnp.ones((HG_CHUNK, HG_CHUNK), dtype=bool))

    def body(state, blk):
        qc, kc, vc, gc = blk
        b = jnp.cumsum(gc, axis=2)
        b_last = b[:, :, -1:, :]
        k_end = kc * jnp.exp(b_last - b)
        new_state = jnp.exp(b_last[:, :, 0, :])[..., None] * state + jnp.einsum('bhsk,bhsv->bhkv', k_end, vc)
        if not readout:
            return new_state, None
        diff = b[:, :, :, None, :] - b[:, :, None, :, :]
        decay = jnp.exp(jnp.where(causal[:, :, None], diff, -jnp.inf))
        scores = jnp.einsum('bhtk,bhsk,bhtsk->bhts', qc, kc, decay)
        o = jnp.einsum('bhts,bhsv->bhtv', scores, vc) + jnp.einsum('bhtk,bhkv->bhtv', qc * jnp.exp(b), state)
        return new_state, o

    final, o = lax.scan(body, s0, (to_chunks(q), to_chunks(k), to_chunks(v), to_chunks(g)))
    if readout:
        o = o.transpose(1, 0, 3, 2, 4).reshape(bsz, length, heads, -1)
    return o, final


def _hgrn2_mixer(hl, hc, w_in, w_out, gnorm_g, lb, with_ctx):
    f32 = jnp.float32
    bsz = hl.shape[0]
    lb = lb.reshape(HG_HEADS, HG_DK)
    log_lb = jnp.log(lb)
    log_keep = jnp.log1p(-lb)
    keep = 1.0 - lb

    def prep(h):
        n = h.shape[1]
        q, i, zf, zb, og = jnp.split(h @ w_in, 5, axis=-1)
        heads = (bsz, n, HG_HEADS, HG_DK)
        q = jax.nn.silu(q.astype(f32)).reshape(heads) * (HG_DK ** -0.5)
        v = i.astype(f32).reshape(bsz, n, HG_HEADS, HG_DV)
        dirs = []
        for z in (zf, zb):
            z = z.astype(f32).reshape(heads)
            log_f = jnp.logaddexp(log_lb, log_keep + jax.nn.log_sigmoid(z))
            k = keep * jax.nn.sigmoid(-z)
            dirs.append((k, log_f))
        return q, v, dirs, og

    def run(q, v, kg, s0, readout, reverse):
        k, g = kg
        if reverse:
            q, k, v, g = (jnp.flip(t, axis=1) for t in (q, k, v, g))
        o, s_final = _hgrn_scan(q, k, v, g, s0, readout)
        if readout and reverse:
            o = jnp.flip(o, axis=1)
        return o, s_final

    def readout_proj(o, og):
        n = o.shape[1]
        o = o * lax.rsqrt(jnp.mean(o * o, axis=-1, keepdims=True) + RMS_EPS) * gnorm_g.astype(f32)
        o = o.reshape(bsz, n, HG_HEADS * HG_DV).astype(hl.dtype) * jax.nn.silu(og)
        return o @ w_out

    qc, vc, dc, ogc = prep(hc)
    ql, vl, dl, ogl = prep(hl)
    zero = jnp.zeros((bsz, HG_HEADS, HG_DK, HG_DV), f32)
    oc_f, sc_f = run(qc, vc, dc[0], zero, with_ctx, False)
    oc_b, sc_b = run(qc, vc, dc[1], zero, with_ctx, True)
    ol_f, _ = run(ql, vl, dl[0], sc_f, True, False)
    ol_b, _ = run(ql, vl, dl[1], sc_b, True, True)
    out_l = readout_proj(ol_f + ol_b, ogl)
    out_c = readout_proj(oc_f + oc_b, ogc) if with_ctx else None
    return out_l, out_c


def _s5_zoh(a_re, a_im, log_dt, b_re, b_im):
    f32 = jnp.float32
    a_re, a_im = a_re.astype(f32), a_im.astype(f32)
    b_re, b_im = b_re.astype(f32), b_im.astype(f32)
    dt = jnp.exp(log_dt.astype(f32))[:, None]
    dta_re, dta_im = dt * a_re, dt * a_im
    mag = jnp.exp(dta_re)
    num_re = mag * jnp.cos(dta_im) - 1.0
    num_im = mag * jnp.sin(dta_im)
    den = a_re * a_re + a_im * a_im
    z_re = (num_re * a_re + num_im * a_im) / den
    z_im = (num_im * a_re - num_re * a_im) / den
    bb_re = z_re[..., None] * b_re - z_im[..., None] * b_im
    bb_im = z_re[..., None] * b_im + z_im[..., None] * b_re
    return dta_re, dta_im, bb_re, bb_im


def _complex_affine_combine(e1, e2):
    a1r, a1i, b1r, b1i = e1
    a2r, a2i, b2r, b2i = e2
    return (a2r * a1r - a2i * a1i, a2r * a1i + a2i * a1r,
            a2r * b1r - a2i * b1i + b2r, a2r * b1i + a2i * b1r + b2i)


def _s5_scan(u, dta_re, dta_im, bb_re, bb_im, c_re, c_im, x0_re, x0_im, readout):
    bsz, length = u.shape[:2]
    n_chunks = length // S5_CHUNK
    uc = u.reshape(bsz, n_chunks, S5_CHUNK, S5_GROUPS, S5_GROUP).transpose(1, 0, 2, 3, 4)
    mag = jnp.exp(dta_re)
    shape = (bsz, S5_CHUNK, S5_GROUPS, S5_STATE)
    a_re = jnp.broadcast_to(mag * jnp.cos(dta_im), shape)
    a_im = jnp.broadcast_to(mag * jnp.sin(dta_im), shape)

    def body(carry, u_blk):
        x0r, x0i = carry
        bu_re = jnp.einsum('bcgh,gph->bcgp', u_blk, bb_re)
        bu_im = jnp.einsum('bcgh,gph->bcgp', u_blk, bb_im)
        ar, ai, xr, xi = lax.associative_scan(_complex_affine_combine, (a_re, a_im, bu_re, bu_im), axis=1)
        xr_full = ar * x0r[:, None] - ai * x0i[:, None] + xr
        xi_full = ar * x0i[:, None] + ai * x0r[:, None] + xi
        new = (xr_full[:, -1], xi_full[:, -1])
        if not readout:
            return new, None
        y = jnp.einsum('bcgp,ghp->bcgh', xr_full, c_re) - jnp.einsum('bcgp,ghp->bcgh', xi_full, c_im)
        return new, y

    final, y = lax.scan(body, (x0_re, x0_im), uc)
    if readout:
        y = y.transpose(1, 0, 2, 3, 4).reshape(bsz, length, S5_GROUPS, S5_GROUP)
    return y, final


def _s5_mixer(hl, hc, a_re, a_im, log_dt, b_re, b_im, c_re, c_im, d_skip, w_glu, with_ctx):
    f32 = jnp.float32
    bsz = hl.shape[0]
    params = [_s5_zoh(a_re[d], a_im[d], log_dt[d], b_re, b_im) for d in range(2)]
    c_re, c_im = c_re.astype(f32), c_im.astype(f32)
    d_g = d_skip.astype(f32).reshape(S5_GROUPS, S5_GROUP)

    def groups(h):
        return h.astype(f32).reshape(bsz, h.shape[1], S5_GROUPS, S5_GROUP)

    def run(u, p, x0, readout, reverse):
        if reverse:
            u = jnp.flip(u, axis=1)
        y, xf = _s5_scan(u, *p, c_re, c_im, x0[0], x0[1], readout)
        if readout and reverse:
            y = jnp.flip(y, axis=1)
        return y, xf

    def glu_out(y_f, y_b, u):
        n = u.shape[1]
        y = (y_f + y_b + d_g * u).reshape(bsz, n, D_MODEL).astype(hl.dtype)
        a, g = jnp.split(y @ w_glu, 2, axis=-1)
        return a * jax.nn.sigmoid(g)

    uc, ul = groups(hc), groups(hl)
    zero = (jnp.zeros((bsz, S5_GROUPS, S5_STATE), f32), jnp.zeros((bsz, S5_GROUPS, S5_STATE), f32))
    yc_f, xc_f = run(uc, params[0], zero, with_ctx, False)
    yc_b, xc_b = run(uc, params[1], zero, with_ctx, True)
    yl_f, _ = run(ul, params[0], xc_f, True, False)
    yl_b, _ = run(ul, params[1], xc_b, True, True)
    out_l = glu_out(yl_f, yl_b, ul)
    out_c = glu_out(yc_f, yc_b, uc) if with_ctx else None
    return out_l, out_c


def _axial_rope(t, rows, cols):
    half = ATT_HEAD_DIM // 2
    quarter = half // 2
    inv_freq = ROPE_BASE ** (-jnp.arange(quarter, dtype=jnp.float32) / quarter)

    def rot(th, pos):
        ang = pos.astype(jnp.float32)[:, None] * inv_freq
        cos = jnp.cos(ang)[None, :, None, :].astype(t.dtype)
        sin = jnp.sin(ang)[None, :, None, :].astype(t.dtype)
        t1, t2 = th[..., :quarter], th[..., quarter:]
        return jnp.concatenate([t1 * cos - t2 * sin, t1 * sin + t2 * cos], axis=-1)

    return jnp.concatenate([rot(t[..., :half], rows), rot(t[..., half:], cols)], axis=-1)


def _sink_softmax(s, sink):
    sk = sink[None, :, :, None, None]
    m = jnp.maximum(jnp.max(s, axis=-1, keepdims=True), sk)
    e = jnp.exp(s - m)
    return e / (jnp.sum(e, axis=-1, keepdims=True) + jnp.exp(sk - m))


def _attn_mixer(hl, hc, w_qkv, w_o, sink, rows, cols, with_ctx):
    bsz, length, _ = hl.shape
    G, R, HD = ATT_KV_HEADS, ATT_HEADS // ATT_KV_HEADS, ATT_HEAD_DIM
    scale = HD ** -0.5
    sink = sink.astype(jnp.float32).reshape(G, R)

    def proj(h):
        n = h.shape[1]
        q, k, v = jnp.split(h @ w_qkv, [ATT_HEADS * HD, (ATT_HEADS + G) * HD], axis=-1)
        return q.reshape(bsz, n, ATT_HEADS, HD), k.reshape(bsz, n, G, HD), v.reshape(bsz, n, G, HD)

    ql, kl, vl = proj(hl)
    qc, kc, vc = proj(hc)
    n_ctx = hc.shape[1]
    ql = _axial_rope(ql, rows, cols).reshape(bsz, length, G, R, HD)
    kl = _axial_rope(kl, rows, cols)
    pad = ((0, 0), (ATT_BLOCK, ATT_BLOCK), (0, 0), (0, 0))
    k_pad, v_pad = jnp.pad(kl, pad), jnp.pad(vl, pad)
    offs_q = jnp.arange(ATT_BLOCK)
    offs_k = jnp.arange(3 * ATT_BLOCK) - ATT_BLOCK

    def block(n):
        start = n * ATT_BLOCK
        q_blk = lax.dynamic_slice_in_dim(ql, start, ATT_BLOCK, axis=1)
        k_win = lax.dynamic_slice_in_dim(k_pad, start, 3 * ATT_BLOCK, axis=1)
        v_win = lax.dynamic_slice_in_dim(v_pad, start, 3 * ATT_BLOCK, axis=1)
        q_pos = start + offs_q
        k_pos = start + offs_k
        valid = (k_pos[None, :] >= 0) & (k_pos[None, :] < length) & (jnp.abs(q_pos[:, None] - k_pos[None, :]) <= ATT_WINDOW)
        s_loc = jnp.einsum('bqgrd,bkgd->bgrqk', q_blk, k_win).astype(jnp.float32) * scale
        s_loc = jnp.where(valid, s_loc, -jnp.inf)
        s_ctx = jnp.einsum('bqgrd,bkgd->bgrqk', q_blk, kc).astype(jnp.float32) * scale
        p = _sink_softmax(jnp.concatenate([s_ctx, s_loc], axis=-1), sink).astype(hl.dtype)
        return (jnp.einsum('bgrqk,bkgd->bqgrd', p[..., :n_ctx], vc)
                + jnp.einsum('bgrqk,bkgd->bqgrd', p[..., n_ctx:], v_win))

    o = lax.map(block, jnp.arange(length // ATT_BLOCK))
    o = o.transpose(1, 0, 2, 3, 4, 5).reshape(bsz, length, ATT_HEADS * HD)
    out_l = o @ w_o
    out_c = None
    if with_ctx:
        qc = qc.reshape(bsz, n_ctx, G, R, HD)
        s = jnp.einsum('bqgrd,bkgd->bgrqk', qc, kc).astype(jnp.float32) * scale
        p = _sink_softmax(s, sink).astype(hc.dtype)
        oc = jnp.einsum('bgrqk,bkgd->bqgrd', p, vc).reshape(bsz, n_ctx, ATT_HEADS * HD)
        out_c = oc @ w_o
    return out_l, out_c


def _hier_moe(h, w_group, b_group, w_expert, b_expert, w_gate_up, w_down):
    f32 = jnp.float32
    h32 = h.astype(f32)
    p_group = jax.nn.softmax(h32 @ w_group.astype(f32) + b_group.astype(f32), axis=-1)
    g_sel = jnp.argmax(p_group, axis=-1)
    p_sel = jnp.max(p_group, axis=-1)
    logits = (h32 @ w_expert.astype(f32) + b_expert.astype(f32)).reshape(
        h.shape[:-1] + (MOE_GROUPS, MOE_EXPERTS_PER_GROUP))
    logits_g = jnp.take_along_axis(logits, g_sel[..., None, None], axis=-2)[..., 0, :]
    top_v, top_i = lax.top_k(logits_g, MOE_TOP_K)
    w_sel = jax.nn.softmax(top_v, axis=-1) * p_sel[..., None]
    expert_id = g_sel[..., None] * MOE_EXPERTS_PER_GROUP + top_i
    gates = jnp.sum(jax.nn.one_hot(expert_id, MOE_EXPERTS, dtype=f32) * w_sel[..., None], axis=-2).astype(h.dtype)
    out = jnp.zeros_like(h)
    for e in range(MOE_EXPERTS):
        a, b = jnp.split(h @ w_gate_up[e], 2, axis=-1)
        out = out + gates[..., e:e + 1] * ((jax.nn.silu(a) * b) @ w_down[e])
    return out


def setup_inputs(seed: int = 0) -> dict:
    key = jax.random.key(seed)
    keys = iter(jax.random.split(key, 32))
    f32 = jnp.float32
    D = D_MODEL

    def normal(shape, std):
        return std * jax.random.normal(next(keys), shape, f32)

    G, P, CH = S5_GROUPS, S5_STATE, S5_GROUP
    qkv_width = (ATT_HEADS + 2 * ATT_KV_HEADS) * ATT_HEAD_DIM
    x = normal((BATCH, SEQ, D), 1.0)
    c = normal((BATCH, D), 1.0)
    ctx = normal((BATCH, CTX_LEN, D), 1.0)
    c_ctx = normal((D,), 1.0)
    ada_w = normal((DEPTH, D, 6 * D), 0.5 * D ** -0.5)
    ada_b = normal((DEPTH, 6 * D), 0.02)
    norm_mix_g = 1.0 + normal((DEPTH, D), 0.05)
    norm_ffn_g = 1.0 + normal((DEPTH, D), 0.05)
    final_norm_g = 1.0 + normal((D,), 0.05)
    hgrn_w_in = normal((N_LAYERS_A, D, 5 * D), D ** -0.5)
    hgrn_w_out = normal((N_LAYERS_A, D, D), D ** -0.5)
    hgrn_gnorm_g = 1.0 + normal((N_LAYERS_A, HG_DV), 0.05)
    hgrn_lb_logits = normal((DEPTH, D), 1.0)
    s5_a_re = -0.5 * jnp.exp(normal((N_LAYERS_B, 2, G, P), 0.05))
    s5_a_im = math.pi * jnp.arange(P, dtype=f32) + normal((N_LAYERS_B, 2, G, P), 0.05)
    s5_log_dt = jax.random.uniform(next(keys), (N_LAYERS_B, 2, G), f32,
                                   minval=math.log(S5_DT_MIN), maxval=math.log(S5_DT_MAX))
    s5_b_re = normal((N_LAYERS_B, G, P, CH), (2 * CH) ** -0.5)
    s5_b_im = normal((N_LAYERS_B, G, P, CH), (2 * CH) ** -0.5)
    s5_c_re = normal((N_LAYERS_B, G, CH, P), 0.5)
    s5_c_im = normal((N_LAYERS_B, G, CH, P), 0.5)
    s5_d = normal((N_LAYERS_B, D), 1.0)
    s5_w_glu = normal((N_LAYERS_B, D, 2 * D), D ** -0.5)
    attn_w_qkv = normal((N_LAYERS_C, D, qkv_width), D ** -0.5)
    attn_w_o = normal((N_LAYERS_C, ATT_HEADS * ATT_HEAD_DIM, D), (ATT_HEADS * ATT_HEAD_DIM) ** -0.5)
    attn_sink = normal((N_LAYERS_C, ATT_HEADS), 1.0)
    moe_w_group = normal((DEPTH, D, MOE_GROUPS), D ** -0.5)
    moe_b_group = normal((DEPTH, MOE_GROUPS), 0.01)
    moe_w_expert = normal((DEPTH, D, MOE_EXPERTS), D ** -0.5)
    moe_b_expert = normal((DEPTH, MOE_EXPERTS), 0.01)
    moe_w_gate_up = normal((DEPTH, MOE_EXPERTS, D, 2 * MOE_D_FF), D ** -0.5)
    moe_w_down = normal((DEPTH, MOE_EXPERTS, MOE_D_FF, D), MOE_D_FF ** -0.5)
    return {"x": x, "c": c, "ctx": ctx, "c_ctx": c_ctx,
            "ada_w": ada_w, "ada_b": ada_b, "norm_mix_g": norm_mix_g, "norm_ffn_g": norm_ffn_g,
            "final_norm_g": final_norm_g,
            "hgrn_w_in": hgrn_w_in, "hgrn_w_out": hgrn_w_out, "hgrn_gnorm_g": hgrn_gnorm_g,
            "hgrn_lb_logits": hgrn_lb_logits,
            "s5_a_re": s5_a_re, "s5_a_im": s5_a_im, "s5_log_dt": s5_log_dt, "s5_b_re": s5_b_re,
            "s5_b_im": s5_b_im, "s5_c_re": s5_c_re, "s5_c_im": s5_c_im, "s5_d": s5_d, "s5_w_glu": s5_w_glu,
            "attn_w_qkv": attn_w_qkv, "attn_w_o": attn_w_o, "attn_sink": attn_sink,
            "moe_w_group": moe_w_group, "moe_b_group": moe_b_group, "moe_w_expert": moe_w_expert,
            "moe_b_expert": moe_b_expert, "moe_w_gate_up": moe_w_gate_up, "moe_w_down": moe_w_down}


def reference(x, c, ctx, c_ctx, ada_w, ada_b, norm_mix_g, norm_ffn_g, final_norm_g,
              hgrn_w_in, hgrn_w_out, hgrn_gnorm_g, hgrn_lb_logits,
              s5_a_re, s5_a_im, s5_log_dt, s5_b_re, s5_b_im, s5_c_re, s5_c_im, s5_d, s5_w_glu,
              attn_w_qkv, attn_w_o, attn_sink,
              moe_w_group, moe_b_group, moe_w_expert, moe_b_expert, moe_w_gate_up, moe_w_down):
    seq = x.shape[1]
    n_ctx = ctx.shape[1]
    ROWS = seq // GRID_W
    rows = jnp.repeat(jnp.arange(ROWS), GRID_W)
    cols = jnp.tile(jnp.arange(GRID_W), ROWS)
    lb_cum = jnp.cumsum(jax.nn.softmax(hgrn_lb_logits.astype(jnp.float32), axis=0), axis=0)
    lower_bounds = lb_cum - lb_cum[:1]
    silu_c = jax.nn.silu(c)
    silu_cc = jax.nn.silu(c_ctx)
    xl, xc = x, ctx
    for i in range(DEPTH):
        with_ctx = i < DEPTH - 1
        mod_l = (silu_c @ ada_w[i] + ada_b[i])[:, None, :]
        mod_c = silu_cc @ ada_w[i] + ada_b[i]
        sh1_l, sc1_l, gt1_l, sh2_l, sc2_l, gt2_l = jnp.split(mod_l, 6, axis=-1)
        sh1_c, sc1_c, gt1_c, sh2_c, sc2_c, gt2_c = jnp.split(mod_c, 6, axis=-1)
        hl = _modulate(_rmsnorm(xl, norm_mix_g[i]), sh1_l, sc1_l)
        hc = _modulate(_rmsnorm(xc, norm_mix_g[i]), sh1_c, sc1_c)
        kind, j = i % N_MIXERS, i // N_MIXERS
        if kind == 0:
            ol, oc = _hgrn2_mixer(hl, hc, hgrn_w_in[j], hgrn_w_out[j], hgrn_gnorm_g[j], lower_bounds[i], with_ctx)
        elif kind == 1:
            ol, oc = _s5_mixer(hl, hc, s5_a_re[j], s5_a_im[j], s5_log_dt[j], s5_b_re[j], s5_b_im[j],
                               s5_c_re[j], s5_c_im[j], s5_d[j], s5_w_glu[j], with_ctx)
        else:
            ol, oc = _attn_mixer(hl, hc, attn_w_qkv[j], attn_w_o[j], attn_sink[j], rows, cols, with_ctx)
        xl = xl + gt1_l * ol
        hl = _modulate(_rmsnorm(xl, norm_ffn_g[i]), sh2_l, sc2_l)
        moe_args = (moe_w_group[i], moe_b_group[i], moe_w_expert[i], moe_b_expert[i], moe_w_gate_up[i], moe_w_down[i])
        if with_ctx:
            xc = xc + gt1_c * oc
            hc = _modulate(_rmsnorm(xc, norm_ffn_g[i]), sh2_c, sc2_c)
            f = _hier_moe(jnp.concatenate([hc, hl], axis=1), *moe_args)
            xc = xc + gt2_c * f[:, :n_ctx]
            xl = xl + gt2_l * f[:, n_ctx:]
        else:
            xl = xl + gt2_l * _hier_moe(hl, *moe_args)
    return _rmsnorm(xl, final_norm_g)
```

```python
import numpy as np
from contextlib import ExitStack
from concourse.bass_utils import run_bass_kernel_spmd
import concourse.bass as bass
import concourse.mybir as mybir

F32 = mybir.dt.float32
BF16 = mybir.dt.bfloat16
I32 = mybir.dt.int32
ALU = mybir.AluOpType
AF = mybir.ActivationFunctionType
AX = mybir.AxisListType


class Dep:
    __slots__ = ("w", "r")

    def __init__(self):
        self.w = None
        self.r = []


class Tile:
    def __init__(self, t, dep=None):
        self.t = t
        self.dep = dep or Dep()

    def __getitem__(self, k):
        return self.t[k]


def _dep(x):
    return x.dep if isinstance(x, Tile) else x


class Prog:
    ENG = ("pe", "act", "dve", "pool", "sp")
    NDMA = 12

    def __init__(self, nc, es):
        self.nc = nc
        self.es = es
        self.stream = {e: [] for e in self.ENG}
        self.cnt = {e: 0 for e in self.ENG}
        self.sems = {}
        for e in self.ENG:
            self.sems[e] = es.enter_context(nc.semaphore("s_" + e))
        self.dma_sems = {}
        self.dma_cnt = {}
        self.dma_rr = {}
        for q in ("sp", "act", "pool"):
            self.dma_rr[q] = 0
            for i in range(self.NDMA):
                k = "d_%s_%d" % (q, i)
                self.sems[k] = es.enter_context(nc.semaphore(k))
                self.dma_cnt[k] = 0
        self.seen = {e: {} for e in self.ENG}
        self.ntile = 0
        self.ninstr = 0

    def sb(self, shape, dtype=F32, name=None):
        self.ntile += 1
        name = name or ("t%d" % self.ntile)
        t = self.es.enter_context(self.nc.sbuf_tensor(name, list(shape), dtype))
        return Tile(t)

    def ps(self, shape, dtype=F32, name=None):
        self.ntile += 1
        name = name or ("p%d" % self.ntile)
        t = self.es.enter_context(self.nc.psum_tensor(name, list(shape), dtype))
        return Tile(t)

    def dram(self, name, shape, dtype=F32, kind="Internal"):
        t = self.nc.dram_tensor(name, list(shape), dtype, kind=kind)
        return Tile(t.ap() if hasattr(t, "ap") else t)

    def _wait(self, eng, tok):
        key, val, src = tok
        if src == eng and eng == "pe":
            return
        if self.seen[eng].get(key, 0) >= val:
            return
        self.seen[eng][key] = val
        self.stream[eng].append(("w", key, val))

    def _deps(self, eng, r, w):
        for d in r:
            d = _dep(d)
            if d.w is not None:
                self._wait(eng, d.w)
        for d in w:
            d = _dep(d)
            if d.w is not None and d.w[2] != eng:
                self._wait(eng, d.w)
            elif d.w is not None and d.w[2] == eng and d.w[0].startswith("d_"):
                self._wait(eng, d.w)
            for t in d.r:
                if t[2] != eng or t[0].startswith("d_"):
                    self._wait(eng, t)

    def _mark(self, tok, r, w):
        for d in r:
            d = _dep(d)
            d.r.append(tok)
            if len(d.r) > 6:
                best = {}
                for t in d.r:
                    if t[0] not in best or best[t[0]][1] < t[1]:
                        best[t[0]] = t
                d.r = list(best.values())
        for d in w:
            d = _dep(d)
            d.w = tok
            d.r = []

    def op(self, eng, fn, r=(), w=()):
        self._deps(eng, r, w)
        self.cnt[eng] += 1
        tok = (eng, self.cnt[eng], eng)
        self.stream[eng].append(("o", fn, eng, 1))
        self._mark(tok, r, w)
        self.ninstr += 1
        return tok

    def dma(self, out, in_, r=(), w=(), q="sp", **kw):
        i = self.dma_rr[q]
        self.dma_rr[q] = (i + 1) % self.NDMA
        key = "d_%s_%d" % (q, i)
        if self.dma_cnt[key] > 0:
            self._wait(q, (key, 16 * self.dma_cnt[key], "dma"))
        self._deps(q, r, w)
        self.dma_cnt[key] += 1
        tok = (key, 16 * self.dma_cnt[key], "dma")
        self.stream[q].append(("o", lambda e: e.dma_start(out=out, in_=in_, **kw), key, 16))
        self._mark(tok, r, w)
        self.ninstr += 1
        return tok

    def wait_all(self, eng, toks):
        for t in toks:
            self._wait(eng, t)

    def emit(self):
        nc = self.nc
        sems = self.sems
        streams = self.stream

        def replay(name):
            def f(e):
                for it in streams[name]:
                    if it[0] == "w":
                        e.wait_ge(sems[it[1]], it[2])
                    else:
                        ins = it[1](e)
                        ins.then_inc(sems[it[2]], it[3])
            return f

        with nc.Block() as block:
            block.tensor(replay("pe"))
            block.scalar(replay("act"))
            block.vector(replay("dve"))
            block.gpsimd(replay("pool"))
            block.sync(replay("sp"))


D = 2048
KC = 16
NCTX = 256
LSEQ = 4096
NT = NCTX + LSEQ
EPS = 1e-6
TILES = [(0, 256)] + [(256 + 512 * i, 512) for i in range(8)]
NE = 32
DFF = 512


class Ctx:
    pass


TAPS = {}


def tap(C, name, ap, shape, deps, dt=F32):
    if not C.dbg or name in TAPS:
        return
    d = C.nc.dram_tensor("tap_" + name, list(shape), dt, kind="ExternalOutput").ap()
    TAPS[name] = d
    C.P.dma(d, ap, r=deps, w=[Tile(d)])


def barrier(P):
    toks = [(e, P.cnt[e], e) for e in P.ENG if P.cnt[e] > 0]
    toks += [(k, 16 * c, "dma") for k, c in P.dma_cnt.items() if c > 0]
    for e in P.ENG:
        for t in toks:
            P._wait(e, t)


def build(n_layers=4, dbg=False, stop=None, nlw=4, layers=None):
    nc = bass.Bass("TRN2", target_bir_lowering=False)
    es = ExitStack()
    P = Prog(nc, es)
    C = Ctx()
    C.nc, C.P = nc, P

    def din(name, shape, dt=F32):
        return Tile(nc.dram_tensor(name, list(shape), dt, kind="ExternalInput").ap())

    C.xT0 = din("xT0", [D, NT])
    C.c2 = din("c2", [128, KC, 2])
    C.ada_w = din("ada_w", [4, D, 6 * D])
    C.ada_b = din("ada_b", [4, 128, 96])
    C.gmix = din("gmix", [128, 4 * KC])
    C.gffn = din("gffn", [128, 4 * KC])
    C.gfin = din("gfin", [128, KC])
    C.hg_w_in = din("hgrn_w_in", [2, D, 5 * D])
    C.hg_w_out = din("hgrn_w_out", [2, D, D])
    C.hg_gn = din("hgrn_gn", [2, 128])
    C.hg_lb = din("hgrn_lb", [128, 4, 16])
    C.wr = din("wr", [4, D, 36])
    C.br = din("br", [4, 36])
    C.w_gu = din("moe_w_gate_up", [4, NE, D, 2 * DFF])
    C.w_dn = din("moe_w_down", [4, NE, DFF, D])
    C.s5_a = din("s5_a", [3, 2, 128, 64])
    C.s5_bp = din("s5_bp", [2, 128, 64, 128])
    C.s5_cp = din("s5_cp", [2, 128, 64, 128])
    C.s5_d = din("s5_d", [128, KC])
    C.s5_wglu = din("s5_w_glu", [1, D, 2 * D])
    C.k_tau = din("k_tau", [128, TBK])
    C.w_qkv = din("attn_w_qkv", [1, D, 3072])
    C.w_o = din("attn_w_o", [1, D, D])
    C.sink = din("attn_sink", [1, 32])
    C.k_rope = din("k_rope", [LSEQ, 64])
    C.k_amask = din("k_amask", [128, 256])
    C.k_sel = din("k_sel", [32, NE * 128])
    C.k_cum = din("k_cum", [64, 2 * 64])
    C.k_selb = din("k_selb", [64, 2 * 3])
    C.k_mask = din("k_mask", [CH, 2 * CH])
    C.outT = Tile(nc.dram_tensor("outT", [D, LSEQ], F32, kind="ExternalOutput").ap())
    C.XT = Tile(nc.dram_tensor("XT", [D, NT], F32, kind="Internal").ap())
    C.OF = Tile(nc.dram_tensor("OF", [D, NT], F32, kind="Internal").ap())
    C.OFT = C.OF.t
    C.dbg = dbg
    C.tapdeps = []
    C.WGU = [nc.dram_tensor("WGU%d" % i, [NE, D, 2 * DFF], BF16, kind="Internal").ap() for i in range(2)]
    C.WDN = [nc.dram_tensor("WDN%d" % i, [NE, DFF, D], BF16, kind="Internal").ap() for i in range(2)]
    C.WIN = nc.dram_tensor("WINb", [D, 5 * D], BF16, kind="Internal").ap()
    C.WOUT = nc.dram_tensor("WOUTb", [D, D], BF16, kind="Internal").ap()
    C.WGLU = nc.dram_tensor("WGLUb", [D, 2 * D], BF16, kind="Internal").ap()
    C.WQKV = nc.dram_tensor("WQKVb", [D, 3072], BF16, kind="Internal").ap()
    C.WO = nc.dram_tensor("WOb", [D, D], BF16, kind="Internal").ap()
    C.wdep = {k: Dep() for k in ("win", "wout", "wglu", "wqkv", "wo")}
    C.wgu_dep = [[Dep() for _ in range(NE)] for _ in range(2)]
    C.wdn_dep = [[Dep() for _ in range(NE)] for _ in range(2)]
    C.XTv = C.XT.t.rearrange("(k p) t -> p k t", p=128)
    C.xT0v = C.xT0.t.rearrange("(k p) t -> p k t", p=128)
    C.outTv = C.outT.t.rearrange("(k p) t -> p k t", p=128)
    C.xdep = [Dep() for _ in TILES]

    C.identf = P.sb([128, 128], F32, "identf")
    C.identb = P.sb([128, 128], BF16, "identb")
    C.ones = P.sb([128, 128], F32, "ones")
    C.epsb = P.sb([128, 1], F32, "epsb")
    C.sc2 = P.sb([128, KC, 2], F32, "sc2")
    C.mod = P.sb([128, 96, 2], F32, "mod")
    C.adab = P.sb([128, 96], F32, "adab")
    C.gmix_s = P.sb([128, 4 * KC], F32, "gmix_s")
    C.gffn_s = P.sb([128, 4 * KC], F32, "gffn_s")
    C.gfin_s = P.sb([128, KC], F32, "gfin_s")
    C.AB = P.sb([128, 4, KC, 2], F32, "AB")
    C.AB2 = P.sb([128, 4, KC], F32, "AB2")
    C.psb = [P.ps([128, 512], F32, "bank%d" % i) for i in range(7)]
    C.psbf = P.ps([128, 1024], BF16, "bankbf")

    P.op("pool", lambda e: e.memset(C.identf[:], 1.0), w=[C.identf])
    P.op("pool", lambda e: e.affine_select(C.identf[:], C.identf[:], [[-1, 128]], ALU.is_equal, 0.0,
                                           base=0, channel_multiplier=1), r=[C.identf], w=[C.identf])
    P.op("dve", lambda e: e.tensor_copy(C.identb[:], C.identf[:]), r=[C.identf], w=[C.identb])
    P.op("dve", lambda e: e.memset(C.ones[:], 1.0), w=[C.ones])
    P.op("dve", lambda e: e.memset(C.epsb[:], EPS), w=[C.epsb])
    P.dma(C.sc2[:], C.c2.t[:, :, :], r=[C.c2], w=[C.sc2])
    P.op("act", lambda e: e.activation(C.sc2[:], C.sc2[:], AF.Silu), r=[C.sc2], w=[C.sc2])
    P.dma(C.gmix_s[:], C.gmix.t[:, :], w=[C.gmix_s])
    P.dma(C.gffn_s[:], C.gffn.t[:, :], w=[C.gffn_s])
    P.dma(C.gfin_s[:], C.gfin.t[:, :], w=[C.gfin_s])
    for ti, (t0, n) in enumerate(TILES):
        P.dma(C.XT.t[:, t0:t0 + n], C.xT0.t[:, t0:t0 + n], r=[C.xT0], w=[C.xdep[ti]])

    C.stop = stop
    C.layer_list = list(layers if layers is not None else range(n_layers))
    mixer_precast(C, C.layer_list[0])
    if stop != "mix":
        precast(C, C.layer_list[0])
    for li in (layers if layers is not None else range(n_layers)):
        ada_phase(C, li)
        kind = li % 3
        if kind == 0:
            hgrn_phase(C, li, li // 3, with_ctx=(li < 3))
        elif kind == 1:
            s5_phase(C, li, with_ctx=(li < 3))
        else:
            attn_phase(C, li, with_ctx=(li < 3))
        if dbg:
            d_ = Tile(nc.dram_tensor("xmix%d" % li, [D, NT], F32, kind="ExternalOutput").ap())
            P.dma(d_.t[:, :], C.XT.t[:, :], r=C.xdep, w=[d_])
            barrier(P)
        if stop != "mix":
            moe_phase(C, li, with_ctx=(li < 3))
        if dbg:
            d_ = Tile(nc.dram_tensor("xdbg%d" % li, [D, NT], F32, kind="ExternalOutput").ap())
            P.dma(d_.t[:, :], C.XT.t[:, :], r=C.xdep, w=[d_])
            barrier(P)
    final_phase(C, n_layers)
    for q in ("sp", "pool", "act"):
        for i in range(P.NDMA):
            k = "d_%s_%d" % (q, i)
            if P.dma_cnt[k]:
                P._wait("sp", (k, 16 * P.dma_cnt[k], "dma"))
    P.emit()
    es.close()
    return nc


def ada_phase(C, li):
    P = C.P
    with ExitStack() as es2:
        old = P.es
        P.es = es2
        wb = [P.sb([128, KC, 512], F32, "adaw%d_%d" % (li, i)) for i in range(2)]
        P.dma(C.adab[:], C.ada_b.t[li, :, :], w=[C.adab])
        wv = C.ada_w.t[li].rearrange("(k p) n -> p k n", p=128)
        ps = C.psb[0]
        for g in range(24):
            w = wb[g % 2]
            for h in range(2):
                P.dma(w[:, h * 8:(h + 1) * 8, :], wv[:, h * 8:(h + 1) * 8, g * 512:(g + 1) * 512], w=[w],
                      q=("sp" if h == 0 else "act"))
            for jj in range(4):
                j = g * 4 + jj
                for k in range(KC):
                    P.op("pe", lambda e, w=w, jj=jj, k=k, j=j: e.matmul(
                        ps[:, j * 2:(j + 1) * 2], w[:, k, jj * 128:(jj + 1) * 128], C.sc2[:, k, :],
                        start=(k == 0), stop=(k == KC - 1)), r=[w, C.sc2], w=[ps])
        P.op("dve", lambda e: e.tensor_tensor(
            C.mod[:], ps[:, 0:192].rearrange("p (j c) -> p j c", c=2),
            C.adab[:].unsqueeze(2).to_broadcast([128, 96, 2]), ALU.add), r=[ps, C.adab], w=[C.mod])
        for ni in range(4):
            c = ni % 2
            base = 0 if ni < 2 else 3
            g = (C.gmix_s if ni < 2 else C.gffn_s)
            sh = C.mod[:, (base + 0) * 16:(base + 1) * 16, c]
            sc = C.mod[:, (base + 1) * 16:(base + 2) * 16, c]
            P.op("dve", lambda e, ni=ni, sc=sc, g=g: e.scalar_tensor_tensor(
                C.AB[:, ni, :, 0], sc, 1.0, g[:, li * 16:(li + 1) * 16], ALU.add, ALU.mult),
                r=[C.mod, g], w=[C.AB])
            P.op("dve", lambda e, ni=ni, sh=sh: e.tensor_copy(C.AB[:, ni, :, 1], sh), r=[C.mod], w=[C.AB])
        barrier(P)
        P.es = old
    barrier(P)


def gate_ap(C, which, c):
    base = 2 if which == 0 else 5
    return C.mod[:, base * 16:(base + 1) * 16, c]


def load_x(C, xbuf, ti, q="sp"):
    t0, n = TILES[ti]
    P = C.P
    for h in range(2):
        P.dma(xbuf[:, h * 8:(h + 1) * 8, :n], C.XTv[:, h * 8:(h + 1) * 8, t0:t0 + n], r=[C.xdep[ti]], w=[xbuf],
              q=("sp" if h == 0 else "act"))


def store_x(C, xbuf, ti):
    t0, n = TILES[ti]
    P = C.P
    for h in range(2):
        P.dma(C.XTv[:, h * 8:(h + 1) * 8, t0:t0 + n], xbuf[:, h * 8:(h + 1) * 8, :n], r=[xbuf], w=[C.xdep[ti]],
              q="sp")


def norm_mod(C, xbuf, n, ni, hbf, tmp, rstd, h32=None, psbank=7):
    P = C.P
    ps = C.psb[psbank]
    for k in range(KC):
        t = tmp[k % len(tmp)]
        P.op("act", lambda e, t=t, k=k: e.activation(t[:, :n], xbuf[:, k, :n], AF.Square), r=[xbuf], w=[t])
        P.op("pe", lambda e, t=t, k=k: e.matmul(ps[:, :n], C.ones[:], t[:, :n], start=(k == 0), stop=(k == KC - 1)),
             r=[t, C.ones], w=[ps])
    P.op("act", lambda e: e.activation(rstd[:, :n], ps[:, :n], AF.Sqrt, bias=C.epsb[:, 0:1], scale=1.0 / D),
         r=[ps, C.epsb], w=[rstd])
    P.op("dve", lambda e: e.reciprocal(rstd[:, :n], rstd[:, :n]), r=[rstd], w=[rstd])
    for k in range(KC):
        t = tmp[k % len(tmp)]
        P.op("dve", lambda e, t=t, k=k: e.tensor_tensor(t[:, :n], xbuf[:, k, :n], rstd[:, :n], ALU.mult),
             r=[xbuf, rstd], w=[t])
        if h32 is not None:
            P.op("act", lambda e, t=t, k=k: e.activation(h32[:, k, :n], t[:, :n], AF.Identity,
                                                         bias=C.AB[:, ni, k, 1:2], scale=C.AB[:, ni, k, 0:1]),
                 r=[t, C.AB], w=[h32])
            P.op("pool", lambda e, k=k: e.tensor_copy(hbf[:, k, :n], h32[:, k, :n]), r=[h32], w=[hbf])
        else:
            P.op("act", lambda e, t=t, k=k: e.activation(hbf[:, k, :n], t[:, :n], AF.Identity,
                                                         bias=C.AB[:, ni, k, 1:2], scale=C.AB[:, ni, k, 0:1]),
                 r=[t, C.AB], w=[hbf])


def hgrn_phase(C, li, j, with_ctx):
    P = C.P
    with ExitStack() as es2:
        old = P.es
        P.es = es2
        S = Ctx()
        S.xbuf = P.sb([128, KC, 512], F32)
        S.hbf = P.sb([128, KC, 512], BF16)
        S.tmp = [P.sb([128, 512], F32) for _ in range(2)]
        S.rstd = P.sb([128, 512], F32)
        S.ring = [[P.sb([128, 4, 256], BF16) for _ in range(4)] for _ in range(8)]
        S.state = P.sb([128, 16, 128], F32)
        S.rst = P.sb([128, 512], F32)
        S.mask = P.sb([CH, 2, CH], F32)
        S.lbt = P.sb([128, 4, 16], F32)
        S.lb = P.sb([128, 16], F32)
        S.keep = P.sb([128, 16], F32)
        S.lbw = P.sb([128, 16], F32)
        S.gn = P.sb([128, 1], F32)
        S.oT = P.sb([128, KC, 512], BF16)
        S.qT = [P.sb([128, 512], F32) for _ in range(2)]
        S.vT = [P.sb([128, 512], BF16) for _ in range(2)]
        S.sg = [P.sb([128, 512], F32)] * 2
        S.g = [P.sb([128, 512], F32)] * 2
        S.bb = [P.sb([128, 512], F32)] * 2
        S.bp = [P.sb([128, 512], F32)] * 2
        S.ep = [P.sb([128, 512], F32)] * 2
        S.en = [P.sb([128, 512], F32)] * 2
        S.qt = [P.sb([128, 512], BF16) for _ in range(2)]
        S.kt = [P.sb([128, 512], BF16) for _ in range(2)]
        S.E = [P.sb([128, 3, 32], F32) for _ in range(2)]
        S.ktok = [P.sb([CH, 4096], BF16)] * 2
        S.vtok = [P.sb([CH, 4096], BF16)] * 2
        S.sT = [P.sb([CH, 512], BF16) for _ in range(2)]
        S.kvs = [P.sb([128, 4, 128], F32) for _ in range(2)]
        S.stb = [P.sb([128, 128], BF16) for _ in range(2)]
        S.of = [P.sb([128, 512], F32)] * 2
        S.o = [P.sb([128, 512], F32)] * 2
        S.sog = [P.sb([128, 512], F32)] * 2
        S.ofdep = [[Dep() for _ in range(16)] for _ in TILES]
        S.sdep = [Dep() for _ in range(16)]
        S.oTdep = [Dep() for _ in range(16)]

        P.op("dve", lambda e: e.memset(S.rst[:], 1.0), w=[S.rst])
        P.op("dve", lambda e: e.memset(S.rst[:].rearrange("p (u t) -> p u t", t=CH)[:, :, 0:1], 0.0), w=[S.rst])
        P.dma(S.mask[:], C.k_mask.t[:, :].rearrange("p (a b) -> p a b", a=2), w=[S.mask])
        P.dma(S.gn[:], C.hg_gn.t[j].rearrange("(p o) -> p o", o=1), w=[S.gn])
        P.dma(S.lbt[:], C.hg_lb.t[:, :, :], w=[S.lbt])
        P.op("dve", lambda e: e.tensor_reduce(S.lbw[:], S.lbt[:].rearrange("p l h -> p h l"), AX.X, ALU.max), r=[S.lbt], w=[S.lbw])
        P.op("dve", lambda e: e.tensor_tensor(S.lbt[:], S.lbt[:], S.lbw[:].unsqueeze(1).to_broadcast([128, 4, 16]), ALU.subtract),
             r=[S.lbt, S.lbw], w=[S.lbt])
        P.op("act", lambda e: e.activation(S.lbt[:], S.lbt[:], AF.Exp), r=[S.lbt], w=[S.lbt])
        P.op("dve", lambda e: e.tensor_reduce(S.lbw[:], S.lbt[:].rearrange("p l h -> p h l"), AX.X, ALU.add), r=[S.lbt], w=[S.lbw])
        P.op("dve", lambda e: e.reciprocal(S.lbw[:], S.lbw[:]), r=[S.lbw], w=[S.lbw])
        if li == 0:
            P.op("dve", lambda e: e.memset(S.lb[:], 0.0), w=[S.lb])
        else:
            P.op("dve", lambda e: e.tensor_reduce(S.lb[:], S.lbt[:, 1:li + 1, :].rearrange("p l h -> p h l"), AX.X, ALU.add),
                 r=[S.lbt], w=[S.lb])
            P.op("dve", lambda e: e.tensor_tensor(S.lb[:], S.lb[:], S.lbw[:], ALU.mult), r=[S.lb, S.lbw], w=[S.lb])
        P.op("dve", lambda e: e.tensor_scalar(S.keep[:], S.lb[:], -1.0, 1.0, ALU.mult, ALU.add), r=[S.lb], w=[S.keep])

        win = C.WIN.rearrange("(k p) n -> p k n", p=128)
        wout = C.WOUT.rearrange("(k p) n -> p k n", p=128)
        ringpos = [0]

        def load_group(src, col0, dep):
            buf = S.ring[ringpos[0] % 8]
            ringpos[0] += 1
            for q4 in range(4):
                P.dma(buf[q4][:], src[:, q4 * 4:(q4 + 1) * 4, col0:col0 + 256], r=[dep], w=[buf[q4]], q="sp")
            return buf

        for pas in (0, 1):
            order = list(range(9)) if pas == 0 else [0] + list(range(8, 0, -1))
            types = [0, 1, 2] if pas == 0 else [0, 1, 3, 4]
            P.op("dve", lambda e: e.memset(S.state[:], 0.0), w=S.sdep)
            steps = []
            for ti in order:
                for hg in range(8):
                    steps.append(("h", ti, hg))
                if pas == 1:
                    for mg in range(8):
                        steps.append(("o", ti, mg))

            def issue(st):
                kind_, ti_, g_ = st
                if kind_ == "h":
                    return [load_group(win, ty * 2048 + g_ * 256, C.wdep["win"]) for ty in types]
                return [load_group(wout, g_ * 256, C.wdep["wout"])]

            pend = issue(steps[0])
            for si, st in enumerate(steps):
                kind_, ti, g_ = st
                W = pend
                if si + 1 < len(steps):
                    pend = issue(steps[si + 1])
                t0, n = TILES[ti]
                cs = 1 if ti == 0 else 0
                if kind_ == "h":
                    if g_ == 0:
                        load_x(C, S.xbuf, ti)
                        norm_mod(C, S.xbuf, n, cs, S.hbf, S.tmp, S.rstd, psbank=0)
                    for hh in range(2):
                        hgrn_head(C, S, li, pas, ti, g_ * 2 + hh, hh, W)
                else:
                    if g_ == 0:
                        load_x(C, S.xbuf, ti)
                    Wo = W[0]
                    for mm in range(2):
                        m = g_ * 2 + mm
                        ps = C.psb[m % 2]
                        for k in range(KC):
                            P.op("pe", lambda e, ps=ps, Wo=Wo, k=k, mm=mm, n=n: e.matmul(
                                ps[:, :n], Wo[k // 4][:, k % 4, mm * 128:(mm + 1) * 128], S.oT[:, k, :n],
                                start=(k == 0), stop=(k == KC - 1)), r=[Wo[k // 4]] + S.oTdep, w=[ps])
                        gt = gate_ap(C, 0, cs)
                        P.op("dve", lambda e, ps=ps, m=m, gt=gt, n=n: e.scalar_tensor_tensor(
                            S.xbuf[:, m, :n], ps[:, :n], gt[:, m:m + 1], S.xbuf[:, m, :n], ALU.mult, ALU.add),
                            r=[ps, S.xbuf, C.mod], w=[S.xbuf])
                    if g_ == 7:
                        store_x(C, S.xbuf, ti)
        barrier(P)
        P.es = old
    barrier(P)


CH = 16


def hgrn_head(C, S, li, pas, ti, h, hh, W):
    P = C.P
    t0, n = TILES[ti]
    nu = n // CH
    b = h % 2
    B = C.psb
    ref = CH // 2 - 1 if pas == 0 else CH // 2
    last = CH - 1 if pas == 0 else 0
    OFv = C.OFT

    def proj(ps, Wp):
        for k in range(KC):
            P.op("pe", lambda e, k=k: e.matmul(ps[:, :n], Wp[k // 4][:, k % 4, hh * 128:(hh + 1) * 128], S.hbf[:, k, :n],
                                               start=(k == 0), stop=(k == KC - 1)), r=[Wp[k // 4], S.hbf], w=[ps])

    if pas == 1:
        P.dma(S.of[b][:, :n], OFv[h * 128:(h + 1) * 128, t0:t0 + n], r=[S.ofdep[ti][h]], w=[S.of[b]])
    proj(B[0], W[0])
    proj(B[1], W[1])
    proj(B[2], W[2])
    if pas == 1:
        proj(B[3], W[3])
    qT, vT, sg, g, bb, bp, ep, en, qt, kt, E = S.qT[b], S.vT[b], S.sg[b], S.g[b], S.bb[b], S.bp[b], S.ep[b], S.en[b], S.qt[b], S.kt[b], S.E[b]
    P.op("act", lambda e: e.activation(qT[:, :n], B[0][:, :n], AF.Silu), r=[B[0]], w=[qT])
    P.op("dve", lambda e: e.tensor_copy(vT[:, :n], B[1][:, :n]), r=[B[1]], w=[vT])
    P.op("act", lambda e: e.activation(sg[:, :n], B[2][:, :n], AF.Sigmoid), r=[B[2]], w=[sg])
    P.op("dve", lambda e: e.tensor_scalar(sg[:, :n], sg[:, :n], S.keep[:, h:h + 1], S.lb[:, h:h + 1], ALU.mult, ALU.add),
         r=[sg, S.keep, S.lb], w=[sg])
    P.op("act", lambda e: e.activation(g[:, :n], sg[:, :n], AF.Ln), r=[sg], w=[g])
    P.op("dve", lambda e: e.tensor_scalar(sg[:, :n], sg[:, :n], -1.0, 1.0, ALU.mult, ALU.add), r=[sg], w=[sg])
    P.op("dve", lambda e: e.tensor_tensor_scan(bb[:, :n], S.rst[:, :n], g[:, :n], 0.0, ALU.mult, ALU.add),
         r=[S.rst, g], w=[bb])
    v3 = lambda t: t[:, :n].rearrange("p (u t) -> p u t", t=CH)
    if pas == 1:
        P.op("dve", lambda e: e.tensor_tensor(g[:, :n], g[:, :n], bb[:, :n], ALU.subtract), r=[g, bb], w=[g])
        P.op("dve", lambda e: e.tensor_tensor(v3(g), v3(g), v3(bb)[:, :, CH - 1:CH].to_broadcast([128, nu, CH]), ALU.add),
             r=[g, bb], w=[g])
        bsrc = g
    else:
        bsrc = bb
    P.op("dve", lambda e: e.tensor_tensor(v3(bp), v3(bsrc), v3(bsrc)[:, :, ref:ref + 1].to_broadcast([128, nu, CH]), ALU.subtract),
         r=[bsrc], w=[bp])
    P.op("act", lambda e: e.activation(ep[:, :n], bp[:, :n], AF.Exp), r=[bp], w=[ep])
    P.op("act", lambda e: e.activation(en[:, :n], bp[:, :n], AF.Exp, scale=-1.0), r=[bp], w=[en])
    P.op("act", lambda e: e.activation(E[:, 0, :nu], v3(bsrc)[:, :, ref], AF.Exp), r=[bsrc], w=[E])
    P.op("act", lambda e: e.activation(E[:, 1, :nu], v3(bsrc)[:, :, last], AF.Exp), r=[bsrc], w=[E])
    P.op("act", lambda e: e.activation(E[:, 2, :nu], v3(bp)[:, :, last], AF.Exp), r=[bp], w=[E])
    P.op("dve", lambda e: e.scalar_tensor_tensor(qt[:, :n], qT[:, :n], 128.0 ** -0.5, ep[:, :n], ALU.mult, ALU.mult),
         r=[qT, ep], w=[qt])
    P.op("dve", lambda e: e.tensor_tensor(kt[:, :n], sg[:, :n], en[:, :n], ALU.mult), r=[sg, en], w=[kt])
    ktok, vtok, sT = S.ktok[b], S.vtok[b], S.sT[b]
    for (src, dst) in ((kt, ktok), (vT, vtok)):
        for u0 in range(0, nu, 8):
            for u in range(u0, u0 + 8):
                P.op("pe", lambda e, u=u, src=src: e.transpose(C.psbf[0:CH, (u % 8) * 128:(u % 8 + 1) * 128], src[:, u * CH:(u + 1) * CH], C.identb[:]),
                     r=[src, C.identb], w=[C.psbf])
            P.op("act", lambda e, dst=dst, u0=u0: e.activation(dst[:, u0 * 128:(u0 + 8) * 128], C.psbf[0:CH, 0:1024], AF.Copy), r=[C.psbf], w=[dst])
    for u in range(nu):
        P.op("pe", lambda e, u=u: e.matmul(B[4][0:CH, u * CH:(u + 1) * CH], kt[:, u * CH:(u + 1) * CH], qt[:, u * CH:(u + 1) * CH],
                                           start=True, stop=True), r=[kt, qt], w=[B[4]])
    P.op("dve", lambda e: e.tensor_tensor(sT[:, :n].rearrange("p (u t) -> p u t", t=CH),
                                          B[4][0:CH, :n].rearrange("p (u t) -> p u t", t=CH),
                                          S.mask[:, pas, :].unsqueeze(1).to_broadcast([CH, nu, CH]), ALU.mult),
         r=[B[4], S.mask], w=[sT])
    st = S.state
    ngr = nu // 4
    grs = list(range(ngr)) if pas == 0 else list(range(ngr - 1, -1, -1))
    for gi, gr in enumerate(grs):
        kvs = S.kvs[gi % 2]
        for uu in range(4):
            u = gr * 4 + uu
            P.op("pe", lambda e, u=u, uu=uu: e.matmul(B[6][:, uu * 128:(uu + 1) * 128], ktok[:, u * 128:(u + 1) * 128],
                                                      vtok[:, u * 128:(u + 1) * 128], start=True, stop=True), r=[ktok, vtok], w=[B[6]])
        P.op("dve", lambda e, gr=gr, kvs=kvs: e.tensor_tensor(kvs[:], B[6][:, :].rearrange("p (u v) -> p u v", v=128),
                                                              E[:, 2, gr * 4:gr * 4 + 4].unsqueeze(2).to_broadcast([128, 4, 128]), ALU.mult),
             r=[B[6], E], w=[kvs])
        us = list(range(4)) if pas == 0 else [3, 2, 1, 0]
        for i, uu in enumerate(us):
            u = gr * 4 + uu
            sb_ = S.stb[i % 2]
            P.op("dve", lambda e, u=u, sb_=sb_: e.tensor_scalar(sb_[:], st[:, h, :], E[:, 0, u:u + 1], None, ALU.mult),
                 r=[S.sdep[h], E], w=[sb_])
            P.op("pe", lambda e, u=u: e.matmul(B[5][:, u * CH:(u + 1) * CH], vtok[:, u * 128:(u + 1) * 128], sT[:, u * CH:(u + 1) * CH],
                                               start=True, stop=False), r=[vtok, sT], w=[B[5]])
            P.op("pe", lambda e, u=u, sb_=sb_: e.matmul(B[5][:, u * CH:(u + 1) * CH], sb_[:], qt[:, u * CH:(u + 1) * CH],
                                                        start=False, stop=True), r=[sb_, qt], w=[B[5]])
            P.op("dve", lambda e, u=u, uu=uu, kvs=kvs: e.scalar_tensor_tensor(st[:, h, :], st[:, h, :], E[:, 1, u:u + 1], kvs[:, uu, :], ALU.mult, ALU.add),
                 r=[S.sdep[h], E, kvs], w=[S.sdep[h]])
    if pas == 0:
        P.op("act", lambda e: e.activation(S.o[b][:, :n], B[5][:, :n], AF.Copy), r=[B[5]], w=[S.o[b]])
        P.dma(OFv[h * 128:(h + 1) * 128, t0:t0 + n], S.o[b][:, :n], r=[S.o[b]], w=[S.ofdep[ti][h]], q="act")
    else:
        o, sog = S.o[b], S.sog[b]
        P.op("dve", lambda e: e.tensor_tensor(o[:, :n], B[5][:, :n], S.of[b][:, :n], ALU.add), r=[B[5], S.of[b]], w=[o])
        P.op("act", lambda e: e.activation(ep[:, :n], o[:, :n], AF.Square), r=[o], w=[ep])
        P.op("pe", lambda e: e.matmul(B[4][:, :n], C.ones[:], ep[:, :n], start=True, stop=True), r=[ep, C.ones], w=[B[4]])
        P.op("act", lambda e: e.activation(en[:, :n], B[4][:, :n], AF.Sqrt, bias=C.epsb[:, 0:1], scale=1.0 / 128), r=[B[4], C.epsb], w=[en])
        P.op("dve", lambda e: e.reciprocal(en[:, :n], en[:, :n]), r=[en], w=[en])
        P.op("act", lambda e: e.activation(sog[:, :n], B[3][:, :n], AF.Silu), r=[B[3]], w=[sog])
        P.op("dve", lambda e: e.tensor_tensor(o[:, :n], o[:, :n], en[:, :n], ALU.mult), r=[o, en], w=[o])
        P.op("dve", lambda e: e.scalar_tensor_tensor(S.oT[:, h, :n], o[:, :n], S.gn[:, 0:1], sog[:, :n], ALU.mult, ALU.mult),
             r=[o, S.gn, sog], w=[S.oTdep[h]])


def mixer_precast(C, li):
    P = C.P
    kind = li % 3
    def rows(dst, src, dep, nblk=8):
        n = D // nblk
        for i in range(nblk):
            P.dma(dst[i * n:(i + 1) * n, :], src[i * n:(i + 1) * n, :], w=[dep], q="pool")
    if kind == 0:
        rows(C.WIN, C.hg_w_in.t[li // 3], C.wdep["win"])
        rows(C.WOUT, C.hg_w_out.t[li // 3], C.wdep["wout"], 2)
    elif kind == 1:
        rows(C.WGLU, C.s5_wglu.t[0], C.wdep["wglu"], 4)
    else:
        src = C.w_qkv.t[0]
        for p_ in range(16):
            for j_, hX in enumerate((p_, 16 + p_)):
                for rc in range(4):
                    rs = slice(rc * 512, (rc + 1) * 512)
                    P.dma(C.WQKV[rs, p_ * 128 + j_ * 64:p_ * 128 + (j_ + 1) * 64], src[rs, hX * 64:(hX + 1) * 64], w=[C.wdep["wqkv"]], q="pool")
        for g_ in range(4):
            for j_, hX in enumerate((g_, 4 + g_)):
                for rc in range(4):
                    rs = slice(rc * 512, (rc + 1) * 512)
                    P.dma(C.WQKV[rs, 2048 + g_ * 128 + j_ * 64:2048 + g_ * 128 + (j_ + 1) * 64], src[rs, 2048 + hX * 64:2048 + (hX + 1) * 64],
                          w=[C.wdep["wqkv"]], q="pool")
        for rc in range(4):
            rs = slice(rc * 512, (rc + 1) * 512)
            P.dma(C.WQKV[rs, 2560:3072], src[rs, 2560:3072], w=[C.wdep["wqkv"]], q="pool")
        rows(C.WO, C.w_o.t[0], C.wdep["wo"], 2)


def precast(C, li):
    P = C.P
    st = li % 2
    for e in range(NE):
        for hx in range(2):
            P.dma(C.WGU[st][e, hx * 1024:(hx + 1) * 1024, :], C.w_gu.t[li, e, hx * 1024:(hx + 1) * 1024, :], w=[C.wgu_dep[st][e]], q="pool")
        P.dma(C.WDN[st][e, :, :], C.w_dn.t[li, e, :, :], w=[C.wdn_dep[st][e]], q="pool")


def moe_phase(C, li, with_ctx):
    P = C.P
    B = C.psb
    tiles = list(range(9)) if with_ctx else list(range(1, 9))
    with ExitStack() as es2:
        old = P.es
        P.es = es2
        buf = P.sb([128, KC, 512], F32)
        bd = [Dep() for _ in range(KC)]
        hbf = P.sb([128, KC, 512], BF16)
        wgu = [[P.sb([128, 4, 1024], BF16) for _ in range(4)] for _ in range(2)]
        wdn = [[P.sb([128, 2, 2048], BF16) for _ in range(2)] for _ in range(2)]
        u = [P.sb([128, 4, 512], BF16) for _ in range(2)]
        tS = [P.sb([128, 512], F32) for _ in range(2)]
        tT = [P.sb([128, 512], F32) for _ in range(2)]
        rstd = P.sb([128, 512], F32)
        wr_s = P.sb([128, KC, 36], F32)
        br_s = P.sb([128, 36], F32)
        gatesT = P.sb([32, 512], F32)
        gm = [P.sb([32, 512], F32) for _ in range(2)]
        rt = P.sb([128, 192], F32)
        xs = [P.sb([128, 4, 512], F32) for _ in range(2)]
        P.dma(wr_s[:], C.wr.t[li].rearrange("(k p) n -> p k n", p=128), w=[wr_s])
        P.dma(br_s[:], C.br.t[li:li + 1, :].partition_broadcast(128), w=[br_s])
        seq = [(ti, e) for ti in tiles for e in range(NE)]

        def issue_w(idx):
            ti, e = seq[idx]
            b = idx % 2
            st = li % 2
            gv = C.WGU[st][e].rearrange("(k p) n -> p k n", p=128)
            dv = C.WDN[st][e].rearrange("(j p) n -> p j n", p=128)
            for q4 in range(4):
                P.dma(wgu[b][q4][:], gv[:, q4 * 4:(q4 + 1) * 4, :], r=[C.wgu_dep[st][e]], w=[wgu[b][q4]], q="sp")
            for q2 in range(2):
                P.dma(wdn[b][q2][:], dv[:, q2 * 2:(q2 + 1) * 2, :], r=[C.wdn_dep[st][e]], w=[wdn[b][q2]], q="sp")

        def prologue(ti):
            t0, n = TILES[ti]
            cs = 1 if ti == 0 else 0
            ni = 3 if ti == 0 else 2
            for hlf in range(2):
                P.dma(buf[:, hlf * 8:(hlf + 1) * 8, :n], C.XTv[:, hlf * 8:(hlf + 1) * 8, t0:t0 + n], r=[C.xdep[ti]],
                      w=bd[hlf * 8:(hlf + 1) * 8])
            for k in range(KC):
                t = tS[k % 2]
                P.op("act", lambda e, t=t, k=k: e.activation(t[:, :n], buf[:, k, :n], AF.Square), r=[bd[k]], w=[t])
                P.op("pe", lambda e, t=t, k=k: e.matmul(B[0][:, :n], C.ones[:], t[:, :n], start=(k == 0), stop=(k == KC - 1)),
                     r=[t, C.ones], w=[B[0]])
            P.op("act", lambda e: e.activation(rstd[:, :n], B[0][:, :n], AF.Sqrt, bias=C.epsb[:, 0:1], scale=1.0 / D),
                 r=[B[0], C.epsb], w=[rstd])
            P.op("dve", lambda e: e.reciprocal(rstd[:, :n], rstd[:, :n]), r=[rstd], w=[rstd])
            for k in range(KC):
                P.op("dve", lambda e, k=k: e.tensor_tensor(buf[:, k, :n], buf[:, k, :n], rstd[:, :n], ALU.mult),
                     r=[bd[k], rstd], w=[bd[k]])
                P.op("act", lambda e, k=k: e.activation(buf[:, k, :n], buf[:, k, :n], AF.Identity,
                                                        bias=C.AB[:, ni, k, 1:2], scale=C.AB[:, ni, k, 0:1]),
                     r=[bd[k], C.AB], w=[bd[k]])
                P.op("dve", lambda e, k=k: e.tensor_copy(hbf[:, k, :n], buf[:, k, :n]), r=[bd[k]], w=[hbf])
            for s in range(n // 128):
                for k in range(KC):
                    P.op("pe", lambda e, k=k, s=s: e.matmul(B[1][:, 0:36], buf[:, k, s * 128:(s + 1) * 128], wr_s[:, k, :],
                                                            start=(k == 0), stop=(k == KC - 1)), r=[bd[k], wr_s], w=[B[1]])
                route(s)

        def route(s):
            R = lambda a, b_: rt[:, a:b_]
            lg, gmax, ngm, eg, gsum, psel = R(0, 36), R(36, 37), R(37, 38), R(38, 42), R(42, 43), R(43, 44)
            ohg, lsel, logg, m1, oh1 = R(44, 48), R(48, 80), R(80, 88), R(88, 89), R(89, 97)
            msk, m2, oh2, dd, e2, w1, w2 = R(97, 105), R(105, 106), R(106, 114), R(114, 115), R(115, 116), R(116, 117), R(117, 118)
            gin, gates = R(118, 126), R(128, 160)
            D1 = lambda fn, rr=(): P.op("dve", fn, r=[rt] + list(rr), w=[rt])
            A1 = lambda fn: P.op("act", fn, r=[rt], w=[rt])
            D1(lambda e: e.tensor_tensor(lg, B[1][:, 0:36], br_s[:], ALU.add), [B[1], br_s])
            D1(lambda e: e.reduce_max(gmax, lg[:, 0:4], AX.X))
            D1(lambda e: e.tensor_scalar(ngm, gmax, -1.0, None, ALU.mult))
            A1(lambda e: e.activation(eg, lg[:, 0:4], AF.Exp, bias=ngm, scale=1.0))
            D1(lambda e: e.reduce_sum(gsum, eg, AX.X))
            D1(lambda e: e.reciprocal(psel, gsum))
            D1(lambda e: e.tensor_scalar(ohg, lg[:, 0:4], gmax, None, ALU.is_equal))
            D1(lambda e: e.tensor_tensor(lsel.rearrange("p (g x) -> p g x", g=4), lg[:, 4:36].rearrange("p (g x) -> p g x", g=4),
                                         ohg.unsqueeze(2).to_broadcast([128, 4, 8]), ALU.mult))
            D1(lambda e: e.tensor_reduce(logg, lsel.rearrange("p (g x) -> p x g", g=4), AX.X, ALU.add))
            D1(lambda e: e.reduce_max(m1, logg, AX.X))
            D1(lambda e: e.tensor_scalar(oh1, logg, m1, None, ALU.is_equal))
            D1(lambda e: e.scalar_tensor_tensor(msk, oh1, -1e30, logg, ALU.mult, ALU.add))
            D1(lambda e: e.reduce_max(m2, msk, AX.X))
            D1(lambda e: e.tensor_scalar(oh2, msk, m2, None, ALU.is_equal))
            D1(lambda e: e.tensor_tensor(dd, m2, m1, ALU.subtract))
            A1(lambda e: e.activation(e2, dd, AF.Exp))
            D1(lambda e: e.tensor_scalar(w1, e2, 1.0, None, ALU.add))
            D1(lambda e: e.reciprocal(w1, w1))
            D1(lambda e: e.tensor_scalar(w2, w1, -1.0, 1.0, ALU.mult, ALU.add))
            D1(lambda e: e.tensor_tensor(w1, w1, psel, ALU.mult))
            D1(lambda e: e.tensor_tensor(w2, w2, psel, ALU.mult))
            D1(lambda e: e.tensor_scalar(gin, oh1, w1, None, ALU.mult))
            D1(lambda e: e.scalar_tensor_tensor(gin, oh2, w2, gin, ALU.mult, ALU.add))
            D1(lambda e: e.tensor_tensor(gates.rearrange("p (g x) -> p g x", g=4), ohg.unsqueeze(2).to_broadcast([128, 4, 8]),
                                         gin.unsqueeze(1).to_broadcast([128, 4, 8]), ALU.mult))
            P.op("pe", lambda e: e.transpose(B[2][0:32, 0:128], gates, C.identf[:]), r=[rt, C.identf], w=[B[2]])
            P.op("act", lambda e: e.activation(gatesT[:, s * 128:(s + 1) * 128], B[2][0:32, 0:128], AF.Copy), r=[B[2]], w=[gatesT])

        def expert(ti, e, b):
            t0, n = TILES[ti]
            g_ = gm[e % 2]
            P.op("dve", lambda e_: e_.tensor_scalar(g_[:, :n], gatesT[:, :n], C.identf[0:32, e:e + 1], None, ALU.mult),
                 r=[gatesT, C.identf], w=[g_])
            P.op("pe", lambda e_: e_.matmul(B[4][:, :n], C.ones[0:32, :], g_[:, :n], start=True, stop=True), r=[g_, C.ones], w=[B[4]])
            for j in range(4):
                pA, pB = B[(2 * j) % 4], B[(2 * j) % 4 + 1]
                for (ps, c0) in ((pA, j * 128), (pB, 512 + j * 128)):
                    for k in range(KC):
                        P.op("pe", lambda e_, ps=ps, c0=c0, k=k: e_.matmul(ps[:, :n], wgu[b][k // 4][:, k % 4, c0:c0 + 128], hbf[:, k, :n],
                                                                           start=(k == 0), stop=(k == KC - 1)),
                             r=[wgu[b][k // 4], hbf], w=[ps])
                s_, t_ = tS[j % 2], tT[j % 2]
                P.op("act", lambda e_, s_=s_, pA=pA: e_.activation(s_[:, :n], pA[:, :n], AF.Silu), r=[pA], w=[s_])
                P.op("dve", lambda e_, s_=s_, t_=t_, pB=pB: e_.tensor_tensor(t_[:, :n], s_[:, :n], pB[:, :n], ALU.mult), r=[s_, pB], w=[t_])
                P.op("dve", lambda e_, t_=t_, j=j: e_.tensor_tensor(u[b][:, j, :n], t_[:, :n], B[4][:, :n], ALU.mult), r=[t_, B[4]], w=[u[b]])
            for m in range(KC):
                pO = B[5 + m % 2]
                for j in range(4):
                    P.op("pe", lambda e_, pO=pO, j=j, m=m: e_.matmul(pO[:, :n], wdn[b][j // 2][:, j % 2, m * 128:(m + 1) * 128], u[b][:, j, :n],
                                                                     start=(j == 0), stop=(j == 3)), r=[wdn[b][j // 2], u[b]], w=[pO])
                if e == 0:
                    P.op("act", lambda e_, pO=pO, m=m: e_.activation(buf[:, m, :n], pO[:, :n], AF.Copy), r=[pO], w=[bd[m]])
                else:
                    P.op("dve", lambda e_, pO=pO, m=m: e_.tensor_tensor(buf[:, m, :n], buf[:, m, :n], pO[:, :n], ALU.add), r=[pO, bd[m]], w=[bd[m]])

        def epilogue(ti):
            t0, n = TILES[ti]
            cs = 1 if ti == 0 else 0
            gt = gate_ap(C, 1, cs)
            for q4 in range(4):
                x_ = xs[q4 % 2]
                P.dma(x_[:, :, :n], C.XTv[:, q4 * 4:(q4 + 1) * 4, t0:t0 + n], r=[C.xdep[ti]], w=[x_])
                for kk in range(4):
                    k = q4 * 4 + kk
                    P.op("dve", lambda e_, x_=x_, kk=kk, k=k: e_.scalar_tensor_tensor(x_[:, kk, :n], buf[:, k, :n], gt[:, k:k + 1], x_[:, kk, :n],
                                                                                      ALU.mult, ALU.add), r=[bd[k], x_, C.mod], w=[x_])
                P.dma(C.XTv[:, q4 * 4:(q4 + 1) * 4, t0:t0 + n], x_[:, :, :n], r=[x_], w=[C.xdep[ti]])

        nxt = C.layer_list.index(li) + 1
        if nxt < len(C.layer_list):
            mixer_precast(C, C.layer_list[nxt])
            precast(C, C.layer_list[nxt])
        issue_w(0)
        for idx, (ti, e) in enumerate(seq):
            if e == 0:
                prologue(ti)
            if idx + 1 < len(seq):
                issue_w(idx + 1)
            expert(ti, e, idx % 2)
            if e == NE - 1:
                epilogue(ti)
        barrier(P)
        P.es = old
    barrier(P)


def final_phase(C, n_layers):
    P = C.P
    with ExitStack() as es2:
        old = P.es
        P.es = es2
        xbuf = P.sb([128, KC, 512], F32)
        tmp = [P.sb([128, 512], F32) for _ in range(2)]
        rstd = P.sb([128, 512], F32)
        for ti in range(1, 9):
            t0, n = TILES[ti]
            load_x(C, xbuf, ti)
            ps = C.psb[0]
            for k in range(KC):
                t = tmp[k % 2]
                P.op("act", lambda e, t=t, k=k: e.activation(t[:, :n], xbuf[:, k, :n], AF.Square), r=[xbuf], w=[t])
                P.op("pe", lambda e, t=t, k=k: e.matmul(ps[:, :n], C.ones[:], t[:, :n], start=(k == 0), stop=(k == KC - 1)),
                     r=[t, C.ones], w=[ps])
            P.op("act", lambda e: e.activation(rstd[:, :n], ps[:, :n], AF.Sqrt, bias=C.epsb[:, 0:1], scale=1.0 / D),
                 r=[ps, C.epsb], w=[rstd])
            P.op("dve", lambda e: e.reciprocal(rstd[:, :n], rstd[:, :n]), r=[rstd], w=[rstd])
            for k in range(KC):
                P.op("dve", lambda e, k=k: e.scalar_tensor_tensor(xbuf[:, k, :n], xbuf[:, k, :n], C.gfin_s[:, k:k + 1], rstd[:, :n],
                                                                  ALU.mult, ALU.mult), r=[xbuf, C.gfin_s, rstd], w=[xbuf])
            for h in range(2):
                P.dma(C.outTv[:, h * 8:(h + 1) * 8, t0 - NCTX:t0 - NCTX + n], xbuf[:, h * 8:(h + 1) * 8, :n], r=[xbuf], w=[C.outT])
        barrier(P)
        P.es = old


TBK = 32
STILES = [(0, 256)] + [(256 + 256 * i, 256) for i in range(16)]
TWO_PI = 6.283185307179586


def rev_last(ap):
    a = [list(x) for x in ap.ap]
    step, cnt = a[-1]
    a[-1] = [-step, cnt]
    return bass.AP(ap.tensor, ap.offset + (cnt - 1) * step, a)


def s5_phase(C, li, with_ctx):
    P = C.P
    B = C.psb
    TB = TBK
    with ExitStack() as es2:
        old = P.es
        P.es = es2
        xbuf = P.sb([128, KC, 256], F32)
        hbf = P.sb([128, KC, 256], BF16)
        tmp = [P.sb([128, 256], F32)]
        rstd = P.sb([128, 256], F32)
        BT = [P.sb([128, 64, 128], BF16) for _ in range(2)]
        CP = [P.sb([128, 64, 128], BF16) for _ in range(2)]
        COS = P.sb([128, 64, TB], F32)
        SIN = P.sb([128, 64, TB], F32)
        RT = P.sb([128, 64, TB], F32)
        G0 = P.sb([128, 2, 32, TB], F32)
        G1 = P.sb([128, 2, 32, TB], F32)
        TA = P.sb([128, 32, TB], F32)
        TBb = P.sb([128, 32, TB], F32)
        Xb = P.sb([128, 2, 32, TB], BF16)
        Y = P.sb([128, KC, 256], F32)
        ybf = P.sb([128, KC, 256], BF16)
        ring = [[P.sb([128, 4, 256], BF16) for _ in range(4)] for _ in range(2)]
        rp = [0]
        ki = P.sb([128, 1024], I32)
        tau = P.sb([128, TB], F32)
        dsk = P.sb([128, KC], F32)
        sm = [P.sb([128, 64], F32, "s5sm%d" % i) for i in range(16)]
        ar, ai, ldt, dar, dai, mag, sinv, cosv, lr, lim, zr, zi, t1, t2, den, t3 = sm
        xin = P.sb([128, 2, 64], F32)
        lamx = P.sb([128, 2, 64], F32)
        s1 = P.sb([128, 64], F32)
        s2 = P.sb([128, 64], F32)
        yfdep = [Dep() for _ in STILES]
        P.dma(tau[:], C.k_tau.t[:, :], w=[tau])
        P.dma(dsk[:], C.s5_d.t[:, :], w=[dsk])
        for c in range(2):
            for q4 in range(4):
                P.dma(CP[c][:, q4 * 16:(q4 + 1) * 16, :], C.s5_cp.t[c, :, q4 * 16:(q4 + 1) * 16, :], w=[CP[c]], q="pool")
        P.op("dve", lambda e: e.tensor_scalar(CP[1][:], CP[1][:], -1.0, None, ALU.mult), r=[CP[1]], w=[CP[1]])
        wgl = C.WGLU.rearrange("(k p) n -> p k n", p=128)

        def load_group(src, col0):
            buf = ring[rp[0] % 2]
            rp[0] += 1
            for q4 in range(4):
                P.dma(buf[q4][:], src[:, q4 * 4:(q4 + 1) * 4, col0:col0 + 256], r=[C.wdep["wglu"]], w=[buf[q4]], q="sp")
            return buf

        def sincos(ang, F, osin, ocos, scratch):
            for (o, sh) in ((osin, 0.0), (ocos, 0.25)):
                P.op("dve", lambda e, sh=sh: e.tensor_scalar(scratch, ang, 1.0 / TWO_PI, sh, ALU.mult, ALU.add), r=[ang_dep[0]], w=[scr_dep[0]])
                for f0 in range(0, F, 1024):
                    f1 = min(F, f0 + 1024)
                    P.op("dve", lambda e, f0=f0, f1=f1: e.tensor_copy(ki[:, :f1 - f0], scratch[:, f0:f1]), r=[scr_dep[0]], w=[ki])
                    P.op("dve", lambda e, o=o, f0=f0, f1=f1: e.tensor_copy(o[:, f0:f1], ki[:, :f1 - f0]), r=[ki], w=[o_dep[0]])
                P.op("dve", lambda e, o=o: e.tensor_tensor(o, scratch, o, ALU.subtract), r=[scr_dep[0], o_dep[0]], w=[o_dep[0]])
                P.op("dve", lambda e, o=o: e.tensor_scalar(o, o, 0.4999995, -0.4999995, ALU.min, ALU.max), r=[o_dep[0]], w=[o_dep[0]])
                P.op("act", lambda e, o=o: e.activation(o, o, AF.Sin, scale=TWO_PI), r=[o_dep[0]], w=[o_dep[0]])

        setup = Dep()
        ang_dep = [setup]
        scr_dep = [setup]
        o_dep = [setup]
        SD = lambda fn, eng="dve": P.op(eng, fn, r=[setup], w=[setup])
        flat = lambda t: t[:].rearrange("p a b -> p (a b)")

        for d in range(2):
            P.dma(ar[:], C.s5_a.t[0, d], w=[setup])
            P.dma(ai[:], C.s5_a.t[1, d], w=[setup])
            P.dma(ldt[:], C.s5_a.t[2, d], w=[setup])
            SD(lambda e: e.activation(ldt[:], ldt[:], AF.Exp), "act")
            SD(lambda e: e.tensor_tensor(dar[:], ldt[:], ar[:], ALU.mult))
            SD(lambda e: e.tensor_tensor(dai[:], ldt[:], ai[:], ALU.mult))
            SD(lambda e: e.activation(mag[:], dar[:], AF.Exp), "act")
            sincos(dai[:], 64, sinv[:], cosv[:], t3[:])
            SD(lambda e: e.tensor_tensor(lr[:], mag[:], cosv[:], ALU.mult))
            SD(lambda e: e.tensor_tensor(lim[:], mag[:], sinv[:], ALU.mult))
            SD(lambda e: e.tensor_scalar(t1[:], lr[:], -1.0, None, ALU.add))
            SD(lambda e: e.tensor_tensor(den[:], ar[:], ar[:], ALU.mult))
            SD(lambda e: e.tensor_tensor(t2[:], ai[:], ai[:], ALU.mult))
            SD(lambda e: e.tensor_tensor(den[:], den[:], t2[:], ALU.add))
            SD(lambda e: e.reciprocal(den[:], den[:]))
            SD(lambda e: e.tensor_tensor(zr[:], t1[:], ar[:], ALU.mult))
            SD(lambda e: e.tensor_tensor(t2[:], lim[:], ai[:], ALU.mult))
            SD(lambda e: e.tensor_tensor(zr[:], zr[:], t2[:], ALU.add))
            SD(lambda e: e.tensor_tensor(zr[:], zr[:], den[:], ALU.mult))
            SD(lambda e: e.tensor_tensor(zi[:], lim[:], ar[:], ALU.mult))
            SD(lambda e: e.tensor_tensor(t2[:], t1[:], ai[:], ALU.mult))
            SD(lambda e: e.tensor_tensor(zi[:], zi[:], t2[:], ALU.subtract))
            SD(lambda e: e.tensor_tensor(zi[:], zi[:], den[:], ALU.mult))
            SD(lambda e: e.tensor_tensor(TA[:] if False else COS[:], dai[:].unsqueeze(2).to_broadcast([128, 64, TB]),
                                         tau[:].unsqueeze(1).to_broadcast([128, 64, TB]), ALU.mult))
            SD(lambda e: e.tensor_copy(RT[:], COS[:]))
            sincos(flat(RT), 64 * TB, flat(SIN), flat(COS), flat(G0)[:, 0:64 * TB] if False else G0[:].rearrange("p a b c -> p (a b c)"))
            SD(lambda e: e.tensor_copy(RT[:], mag[:].unsqueeze(2).to_broadcast([128, 64, TB])))
            SD(lambda e: e.memset(RT[:, :, 0:1], 0.0))
            for jc in range(4):
                js = slice(jc * 16, (jc + 1) * 16)
                bre = G0[:].rearrange("p a b c -> p (a b c)").rearrange("p (j x) -> p j x", x=128)
                bim = G1[:].rearrange("p a b c -> p (a b c)").rearrange("p (j x) -> p j x", x=128)
                u1 = TA[:].rearrange("p a b -> p (a b)")
                P.dma(bre, C.s5_bp.t[0, :, js, :], w=[setup])
                P.dma(bim, C.s5_bp.t[1, :, js, :], w=[setup])
                o1 = Y[:].rearrange("p a b -> p (a b)")[:, 0:2048].rearrange("p (j x) -> p j x", x=128)
                o2 = Y[:].rearrange("p a b -> p (a b)")[:, 2048:4096].rearrange("p (j x) -> p j x", x=128)
                w1 = xbuf[:].rearrange("p a b -> p (a b)")[:, 0:2048].rearrange("p (j x) -> p j x", x=128)
                zrb = zr[:, js].unsqueeze(2).to_broadcast([128, 16, 128])
                zib = zi[:, js].unsqueeze(2).to_broadcast([128, 16, 128])
                SD(lambda e, zrb=zrb: e.tensor_tensor(o1, bre, zrb, ALU.mult))
                SD(lambda e, zib=zib: e.tensor_tensor(w1, bim, zib, ALU.mult))
                SD(lambda e: e.tensor_tensor(o1, o1, w1, ALU.subtract))
                SD(lambda e, zib=zib: e.tensor_tensor(o2, bre, zib, ALU.mult))
                SD(lambda e, zrb=zrb: e.tensor_tensor(w1, bim, zrb, ALU.mult))
                SD(lambda e: e.tensor_tensor(o2, o2, w1, ALU.add))
                for c, oo in ((0, o1), (1, o2)):
                    for j4 in range(4):
                        for jj in range(4):
                            jl = j4 * 4 + jj
                            P.op("pe", lambda e, oo=oo, jl=jl, jj=jj: e.transpose(B[0][:, jj * 128:(jj + 1) * 128], oo[:, jl, :], C.identf[:]),
                                 r=[setup, C.identf], w=[B[0]])
                        P.op("act", lambda e, c=c, jc=jc, j4=j4: e.activation(BT[c][:, jc * 16 + j4 * 4:jc * 16 + j4 * 4 + 4, :],
                                                                             B[0][:, :].rearrange("p (j x) -> p j x", x=128), AF.Copy),
                             r=[B[0]], w=[BT[c]])
            barrier(P)
            P.op("dve", lambda e: e.memset(xin[:], 0.0), w=[xin])
            P.op("dve", lambda e: e.memset(lamx[:], 0.0), w=[lamx])
            order = list(range(17)) if d == 0 else [0] + list(range(16, 0, -1))
            for si in order:
                t0, n = STILES[si]
                pti = 0 if si == 0 else 1 + (si - 1) // 2
                cs_ = 1 if si == 0 else 0
                for hx in range(2):
                    P.dma(xbuf[:, hx * 8:(hx + 1) * 8, :n], C.XTv[:, hx * 8:(hx + 1) * 8, t0:t0 + n], r=[C.xdep[pti]], w=[xbuf])
                norm_mod(C, xbuf, n, cs_, hbf, tmp, rstd, psbank=0)
                nblk = n // TB
                blks = list(range(nblk)) if d == 0 else list(range(nblk - 1, -1, -1))
                for bi in blks:
                    c0 = bi * TB
                    for hf in range(2):
                        s5_block(C, d, hf, c0, hbf, BT, CP, COS, SIN, RT, G0, G1, TA, TBb, Xb, Y, xin, lamx, lr, lim, s1, s2)
                if d == 0:
                    for hx in range(2):
                        P.dma(C.OFT[:, t0:t0 + n].rearrange("(k p) t -> p k t", p=128)[:, hx * 8:(hx + 1) * 8, :], Y[:, hx * 8:(hx + 1) * 8, :n],
                              r=[Y], w=[yfdep[si]])
                else:
                    for hx in range(2):
                        P.dma(xbuf[:, hx * 8:(hx + 1) * 8, :n], C.OFT[:, t0:t0 + n].rearrange("(k p) t -> p k t", p=128)[:, hx * 8:(hx + 1) * 8, :],
                              r=[yfdep[si]], w=[xbuf])
                    for k in range(KC):
                        P.op("dve", lambda e, k=k, n=n: e.tensor_tensor(Y[:, k, :n], Y[:, k, :n], xbuf[:, k, :n], ALU.add), r=[Y, xbuf], w=[Y])
                        P.op("dve", lambda e, k=k, n=n: e.scalar_tensor_tensor(ybf[:, k, :n], hbf[:, k, :n], dsk[:, k:k + 1], Y[:, k, :n], ALU.mult, ALU.add),
                             r=[Y, hbf, dsk], w=[ybf])
                    for hx in range(2):
                        P.dma(xbuf[:, hx * 8:(hx + 1) * 8, :n], C.XTv[:, hx * 8:(hx + 1) * 8, t0:t0 + n], r=[C.xdep[pti]], w=[xbuf])
                    gt = gate_ap(C, 0, cs_)
                    for mg in range(8):
                        Wa = load_group(wgl, mg * 256)
                        Wg = load_group(wgl, 2048 + mg * 256)
                        for mm in range(2):
                            m = mg * 2 + mm
                            for (Wx, ps) in ((Wa, B[5]), (Wg, B[6])):
                                for k in range(KC):
                                    P.op("pe", lambda e, Wx=Wx, ps=ps, k=k, mm=mm, n=n: e.matmul(ps[:, :n], Wx[k // 4][:, k % 4, mm * 128:(mm + 1) * 128], ybf[:, k, :n],
                                                                                              start=(k == 0), stop=(k == KC - 1)), r=[Wx[k // 4], ybf], w=[ps])
                            P.op("act", lambda e, n=n: e.activation(tmp[0][:, :n], B[6][:, :n], AF.Sigmoid), r=[B[6]], w=[tmp[0]])
                            P.op("dve", lambda e, n=n: e.tensor_tensor(tmp[0][:, :n], tmp[0][:, :n], B[5][:, :n], ALU.mult), r=[tmp[0], B[5]], w=[tmp[0]])
                            P.op("dve", lambda e, m=m, gt=gt, n=n: e.scalar_tensor_tensor(xbuf[:, m, :n], tmp[0][:, :n], gt[:, m:m + 1], xbuf[:, m, :n], ALU.mult, ALU.add),
                                 r=[tmp[0], xbuf, C.mod], w=[xbuf])
                    for hx in range(2):
                        P.dma(C.XTv[:, hx * 8:(hx + 1) * 8, t0:t0 + n], xbuf[:, hx * 8:(hx + 1) * 8, :n], r=[xbuf], w=[C.xdep[pti]])
            barrier(P)
        barrier(P)
        P.es = old
    barrier(P)


def s5_block(C, d, hf, c0, hbf, BT, CP, COS, SIN, RT, G0, G1, TA, TBb, Xb, Y, xin, lamx, lr, lim, s1, s2):
    P = C.P
    B = C.psb
    TB = TBK
    j0 = hf * 32
    js = slice(j0, j0 + 32)
    for c in range(2):
        for jl in range(32):
            j = j0 + jl
            bank = B[c * 2 + jl // 16]
            P.op("pe", lambda e, c=c, j=j, jl=jl, bank=bank: e.matmul(bank[:, (jl % 16) * TB:(jl % 16 + 1) * TB], BT[c][:, j, :], hbf[:, j // 4, c0:c0 + TB],
                                                                      start=True, stop=True), r=[BT[c], hbf], w=[bank])
        for hb in range(2):
            src = B[c * 2 + hb][:, :].rearrange("p (j t) -> p j t", t=TB)
            if d == 1:
                src = rev_last(src)
            P.op("act", lambda e, c=c, hb=hb, src=src: e.activation(G0[:, c, hb * 16:(hb + 1) * 16, :], src, AF.Copy), r=[B[c * 2 + hb]], w=[G0])
    cs, sn, rt = COS[:, js, :], SIN[:, js, :], RT[:, js, :]
    bur, bui = G0[:, 0], G0[:, 1]
    vr, vi = G1[:, 0], G1[:, 1]
    D = lambda fn, r, w: P.op("dve", fn, r=r, w=w)
    D(lambda e: e.tensor_tensor(TA[:], bur, cs, ALU.mult), [G0, COS], [TA])
    D(lambda e: e.tensor_tensor(TBb[:], bui, sn, ALU.mult), [G0, SIN], [TBb])
    D(lambda e: e.tensor_tensor(vr, TA[:], TBb[:], ALU.add), [TA, TBb], [G1])
    D(lambda e: e.tensor_tensor(TA[:], bui, cs, ALU.mult), [G0, COS], [TA])
    D(lambda e: e.tensor_tensor(TBb[:], bur, sn, ALU.mult), [G0, SIN], [TBb])
    D(lambda e: e.tensor_tensor(vi, TA[:], TBb[:], ALU.subtract), [TA, TBb], [G1])
    D(lambda e: e.tensor_tensor(G1[:, :, :, 0], G1[:, :, :, 0], lamx[:, :, js], ALU.add), [G1, lamx], [G1])
    fl = lambda a: a.rearrange("p j t -> p (j t)")
    D(lambda e: e.tensor_tensor_scan(fl(bur), fl(rt), fl(vr), 0.0, ALU.mult, ALU.add), [RT, G1], [G0])
    D(lambda e: e.tensor_tensor_scan(fl(bui), fl(rt), fl(vi), 0.0, ALU.mult, ALU.add), [RT, G1], [G0])
    D(lambda e: e.tensor_tensor(TA[:], bur, cs, ALU.mult), [G0, COS], [TA])
    D(lambda e: e.tensor_tensor(TBb[:], bui, sn, ALU.mult), [G0, SIN], [TBb])
    D(lambda e: e.tensor_tensor(vr, TA[:], TBb[:], ALU.subtract), [TA, TBb], [G1])
    D(lambda e: e.tensor_tensor(TA[:], bur, sn, ALU.mult), [G0, SIN], [TA])
    D(lambda e: e.tensor_tensor(TBb[:], bui, cs, ALU.mult), [G0, COS], [TBb])
    D(lambda e: e.tensor_tensor(vi, TA[:], TBb[:], ALU.add), [TA, TBb], [G1])
    P.op("act", lambda e: e.activation(Xb[:, 0], vr, AF.Copy), r=[G1], w=[Xb])
    P.op("act", lambda e: e.activation(Xb[:, 1], vi, AF.Copy), r=[G1], w=[Xb])
    D(lambda e: e.tensor_copy(xin[:, :, js], G1[:, :, :, TB - 1]), [G1], [xin])
    D(lambda e: e.tensor_tensor(s1[:, js], lr[:, js], xin[:, 0, js], ALU.mult), [xin, lr], [s1])
    D(lambda e: e.tensor_tensor(s2[:, js], lim[:, js], xin[:, 1, js], ALU.mult), [xin, lim], [s2])
    D(lambda e: e.tensor_tensor(lamx[:, 0, js], s1[:, js], s2[:, js], ALU.subtract), [s1, s2], [lamx])
    D(lambda e: e.tensor_tensor(s1[:, js], lr[:, js], xin[:, 1, js], ALU.mult), [xin, lr], [s1])
    D(lambda e: e.tensor_tensor(s2[:, js], lim[:, js], xin[:, 0, js], ALU.mult), [xin, lim], [s2])
    D(lambda e: e.tensor_tensor(lamx[:, 1, js], s1[:, js], s2[:, js], ALU.add), [s1, s2], [lamx])
    for kk in range(8):
        kc = hf * 8 + kk
        for jj in range(4):
            for c in range(2):
                jl = kk * 4 + jj
                P.op("pe", lambda e, kk=kk, jl=jl, c=c: e.matmul(B[4][:, kk * TB:(kk + 1) * TB], CP[c][:, j0 + jl, :], Xb[:, c, jl, :],
                                                                 start=(jj == 0 and c == 0), stop=(jj == 3 and c == 1)), r=[CP[c], Xb], w=[B[4]]) \
                    if False else P.op("pe", (lambda kk, jl, c, jj: (lambda e: e.matmul(B[4][:, kk * TB:(kk + 1) * TB], CP[c][:, j0 + jl, :], Xb[:, c, jl, :],
                                                                                start=(jj == 0 and c == 0), stop=(jj == 3 and c == 1))))(kk, jl, c, jj),
                                       r=[CP[c], Xb], w=[B[4]])
    src = B[4][:, 0:8 * TB].rearrange("p (k t) -> p k t", t=TB)
    if d == 1:
        src = rev_last(src)
    P.op("act", lambda e: e.activation(Y[:, hf * 8:(hf + 1) * 8, c0:c0 + TB], src, AF.Copy), r=[B[4]], w=[Y])


def attn_phase(C, li, with_ctx):
    P = C.P
    B = C.psb
    SC = 64.0 ** -0.5
    NEG = -1.0e4
    with ExitStack() as es2:
        old = P.es
        P.es = es2
        xbuf = P.sb([128, KC, 512], F32)
        hbf = P.sb([128, KC, 512], BF16)
        tmp = [P.sb([128, 512], F32)]
        rstd = P.sb([128, 512], F32)
        kT = P.sb([128, 4, NT], BF16)
        vall = P.sb([128, 34, 512], BF16)
        ring = [[P.sb([128, 4, 512], BF16) for _ in range(4)] for _ in range(2)]
        rp = [0]
        rope = P.sb([128, 2, 2, 16], F32)
        ktok = P.sb([128, 512], F32)
        krot = P.sb([128, 512], BF16)
        rt1 = P.sb([128, 512], F32)
        qtok = P.sb([128, 4, 2048], BF16)
        qT = P.sb([128, 16, 128], BF16)
        ssb = P.sb([128, 640], F32)
        pb = P.sb([128, 640], BF16)
        pT = P.sb([128, 640], BF16)
        otok = P.sb([128, 2048], BF16)
        oT = P.sb([128, KC, 512], BF16)
        sm = P.sb([128, 8], F32)
        sinkb = P.sb([128, 32], F32)
        maskLR = P.sb([128, 2, 128], F32)
        P.dma(sinkb[:], C.sink.t[0:1, :].partition_broadcast(128), w=[sinkb])
        P.dma(maskLR[:], C.k_amask.t[:, :].rearrange("p (a b) -> p a b", a=2), w=[maskLR])
        wv = C.WQKV.rearrange("(k p) n -> p k n", p=128)
        wov = C.WO.rearrange("(k p) n -> p k n", p=128)

        def load_group(src, col0):
            buf = ring[rp[0] % 2]
            rp[0] += 1
            for q4 in range(4):
                P.dma(buf[q4][:], src[:, q4 * 4:(q4 + 1) * 4, col0:col0 + 512], r=[C.wdep["wqkv"], C.wdep["wo"]], w=[buf[q4]], q="sp")
            return buf

        def load_group_perm(src, base, pairs):
            buf = ring[rp[0] % 2]
            rp[0] += 1
            for i, (hA, hB) in enumerate(pairs):
                for q4 in range(4):
                    for j_, hX in enumerate((hA, hB)):
                        P.dma(buf[q4][:, :, i * 128 + j_ * 64:i * 128 + (j_ + 1) * 64],
                              src[:, q4 * 4:(q4 + 1) * 4, base + hX * 64:base + (hX + 1) * 64], w=[buf[q4]], q="pool")
            return buf

        def do_rope(src, dst, nh, blk):
            P.dma(rope[:], C.k_rope.t[blk * 128:(blk + 1) * 128, :].rearrange("p (a b c) -> p a b c", a=2, b=2), w=[rope])
            v = lambda t: t[:, :nh * 64].rearrange("p (h a b c) -> p (h a) b c", a=2, b=2, c=16)
            cs = lambda i: rope[:, i, :, :].unsqueeze(1).to_broadcast([128, nh, 2, 16]).rearrange("p h a c -> p (h a) c") \
                if False else None
            x1 = v(src)[:, :, 0, :]
            x2 = v(src)[:, :, 1, :]
            d1 = v(dst)[:, :, 0, :]
            d2 = v(dst)[:, :, 1, :]
            t1 = v(rt1)[:, :, 0, :]
            t2 = v(rt1)[:, :, 1, :]
            Cc = rope[:, 0, :, :].unsqueeze(1).to_broadcast([128, nh, 2, 16])
            Ss = rope[:, 1, :, :].unsqueeze(1).to_broadcast([128, nh, 2, 16])
            r4 = lambda a: a.rearrange("p (h a) c -> p h a c", a=2)
            P.op("dve", lambda e: e.tensor_tensor(r4(t1), r4(x1), Cc, ALU.mult), r=[src, rope], w=[rt1])
            P.op("dve", lambda e: e.tensor_tensor(r4(t2), r4(x2), Ss, ALU.mult), r=[src, rope], w=[rt1])
            P.op("dve", lambda e: e.tensor_tensor(d1, t1, t2, ALU.subtract), r=[rt1], w=[dst])
            P.op("dve", lambda e: e.tensor_tensor(r4(t1), r4(x1), Ss, ALU.mult), r=[src, rope, dst], w=[rt1])
            P.op("dve", lambda e: e.tensor_tensor(r4(t2), r4(x2), Cc, ALU.mult), r=[src, rope], w=[rt1])
            P.op("dve", lambda e: e.tensor_tensor(d2, t1, t2, ALU.add), r=[rt1], w=[dst])

        for ti in range(9):
            t0, n = TILES[ti]
            load_x(C, xbuf, ti)
            norm_mod(C, xbuf, n, 1 if ti == 0 else 0, hbf, tmp, rstd, psbank=0)
            Wk = load_group(wv, 2048)
            Wv = load_group(wv, 2560)
            for bl in range(n // 128):
                gb = t0 // 128 + bl
                for (Wp, ps) in ((Wk, B[1]), (Wv, B[2])):
                    for k in range(KC):
                        P.op("pe", lambda e, Wp=Wp, ps=ps, k=k, bl=bl: e.matmul(ps[:, :], hbf[:, k, bl * 128:(bl + 1) * 128], Wp[k // 4][:, k % 4, :],
                                                                             start=(k == 0), stop=(k == KC - 1)), r=[hbf, Wp[k // 4]], w=[ps])
                P.op("act", lambda e, gb=gb: e.activation(vall[:, gb, :], B[2][:, :], AF.Copy), r=[B[2]], w=[vall])
                if ti == 0:
                    P.op("act", lambda e: e.activation(krot[:, 0:512], B[1][:, :], AF.Copy), r=[B[1]], w=[krot])
                else:
                    P.op("act", lambda e: e.activation(ktok[:, 0:512], B[1][:, :], AF.Copy), r=[B[1]], w=[ktok])
                    do_rope(ktok, krot, 8, gb - 2)
                for g in range(4):
                    P.op("pe", lambda e, g=g: e.transpose(C.psbf[:, g * 128:(g + 1) * 128],
                                                          krot[:, g * 128:(g + 1) * 128], C.identb[:]),
                         r=[krot, C.identb], w=[C.psbf])
                P.op("act", lambda e, gb=gb: e.activation(kT[:, :, gb * 128:(gb + 1) * 128], C.psbf[:, 0:512].rearrange("p (g t) -> p g t", g=4), AF.Copy),
                     r=[C.psbf], w=[kT])
        for ti in range(9):
            t0, n = TILES[ti]
            cs_ = 1 if ti == 0 else 0
            load_x(C, xbuf, ti)
            norm_mod(C, xbuf, n, cs_, hbf, tmp, rstd, psbank=0)
            for qg in range(4):
                Wq = load_group(wv, qg * 512)
                for bl in range(n // 128):
                    ps = B[qg % 2]
                    for k in range(KC):
                        P.op("pe", lambda e, ps=ps, k=k, bl=bl, Wq=Wq: e.matmul(ps[:, :], hbf[:, k, bl * 128:(bl + 1) * 128], Wq[k // 4][:, k % 4, :],
                                                                         start=(k == 0), stop=(k == KC - 1)), r=[hbf, Wq[k // 4]], w=[ps])
                    if ti == 0:
                        P.op("act", lambda e, ps=ps, bl=bl, qg=qg: e.activation(qtok[:, bl, qg * 512:(qg + 1) * 512], ps[:, :], AF.Copy), r=[ps], w=[qtok])
                    else:
                        P.op("act", lambda e, ps=ps: e.activation(ktok[:, 0:512], ps[:, :], AF.Copy), r=[ps], w=[ktok])
                        do_rope(ktok, krot, 8, (t0 - NCTX) // 128 + bl)
                        P.op("pool", lambda e, bl=bl, qg=qg: e.tensor_copy(qtok[:, bl, qg * 512:(qg + 1) * 512], krot[:, 0:512]), r=[krot], w=[qtok])
            for bl in range(n // 128):
                gb = t0 // 128 + bl
                nb = gb - 2
                for hg in range(2):
                    for hh in range(8):
                        h = hg * 8 + hh
                        P.op("pe", lambda e, h=h, hh=hh, bl=bl: e.transpose(C.psbf[:, hh * 128:(hh + 1) * 128],
                                                                            qtok[:, bl, h * 128:(h + 1) * 128], C.identb[:]),
                             r=[qtok, C.identb], w=[C.psbf])
                    P.op("act", lambda e, hg=hg: e.activation(qT[:, hg * 8:(hg + 1) * 8, :], C.psbf[:, 0:1024].rearrange("p (g t) -> p g t", g=8), AF.Copy),
                         r=[C.psbf], w=[qT])
                if ti == 0:
                    kbs = [(0, None), (1, None)]
                else:
                    kbs = [(0, None), (1, None)]
                    if nb > 0:
                        kbs.append((gb - 1, 0))
                    kbs.append((gb, None))
                    if nb < 31:
                        kbs.append((gb + 1, 1))
                nk = len(kbs) * 128
                for h in range(32):
                    g = h // 4
                    bankA, bankB = B[3], B[4]
                    for i, (kb, mk) in enumerate(kbs):
                        dst = bankA[:, i * 128:(i + 1) * 128] if i < 4 else bankB[:, 0:128]
                        bk = bankA if i < 4 else bankB
                        po = 0 if h < 16 else 64
                        P.op("pe", lambda e, dst=dst, h=h, g=g, kb=kb, po=po: e.matmul(dst, qT[po:po + 64, h % 16, :], kT[po:po + 64, g % 4, kb * 128:(kb + 1) * 128],
                                                                                   start=True, stop=True), r=[qT, kT], w=[bk])
                    for i, (kb, mk) in enumerate(kbs):
                        src = bankA[:, i * 128:(i + 1) * 128] if i < 4 else bankB[:, 0:128]
                        bk = bankA if i < 4 else bankB
                        if mk is None:
                            P.op("act", lambda e, src=src, i=i: e.activation(ssb[:, i * 128:(i + 1) * 128], src, AF.Copy), r=[bk], w=[ssb])
                        else:
                            P.op("dve", lambda e, src=src, i=i, mk=mk: e.tensor_tensor(ssb[:, i * 128:(i + 1) * 128], src, maskLR[:, mk, :], ALU.add),
                                 r=[bk, maskLR], w=[ssb])
                    P.op("dve", lambda e, nk=nk: e.reduce_max(sm[:, 0:1], ssb[:, :nk], AX.X), r=[ssb], w=[sm])
                    P.op("dve", lambda e, h=h: e.tensor_scalar(sm[:, 0:1], sm[:, 0:1], SC, sinkb[:, h:h + 1], ALU.mult, ALU.max), r=[sm, sinkb], w=[sm])
                    P.op("dve", lambda e: e.tensor_scalar(sm[:, 1:2], sm[:, 0:1], -1.0, None, ALU.mult), r=[sm], w=[sm])
                    P.op("dve", lambda e: e.memset(sm[:, 2:3], 0.0), r=[sm], w=[sm])
                    P.op("act", lambda e, nk=nk: e.activation(pb[:, :nk], ssb[:, :nk], AF.Exp, bias=sm[:, 1:2], scale=SC, accum_out=sm[:, 2:3]), r=[ssb, sm], w=[pb, sm])
                    P.op("act", lambda e, h=h: e.activation(sm[:, 3:4], sm[:, 0:1], AF.Exp, bias=sinkb[:, h:h + 1], scale=-1.0), r=[sm, sinkb], w=[sm])
                    P.op("dve", lambda e: e.tensor_tensor(sm[:, 4:5], sm[:, 2:3], sm[:, 3:4], ALU.add), r=[sm], w=[sm])
                    P.op("dve", lambda e: e.reciprocal(sm[:, 5:6], sm[:, 4:5]), r=[sm], w=[sm])
                    for i in range(len(kbs)):
                        P.op("pe", lambda e, i=i: e.transpose(C.psbf[:, i * 128:(i + 1) * 128], pb[:, i * 128:(i + 1) * 128], C.identb[:]),
                             r=[pb, C.identb], w=[C.psbf])
                    P.op("act", lambda e, nk=nk: e.activation(pT[:, :nk], C.psbf[:, :nk], AF.Copy), r=[C.psbf], w=[pT])
                    for i, (kb, mk) in enumerate(kbs):
                        P.op("pe", lambda e, i=i, kb=kb, g=g, nkb=len(kbs): e.matmul(B[5][:, 0:64], pT[:, i * 128:(i + 1) * 128], vall[:, kb, g * 64:(g + 1) * 64],
                                                                       start=(i == 0), stop=(i == nkb - 1)), r=[pT, vall], w=[B[5]])
                    P.op("dve", lambda e, h=h: e.tensor_scalar(otok[:, h * 64:(h + 1) * 64], B[5][:, 0:64], sm[:, 5:6], None, ALU.mult), r=[B[5], sm], w=[otok])
                    if C.dbg and ((ti == 0 and bl == 0) or (ti == 1 and bl == 1)) and h in (0, 17):
                        x_ = "_t%d_h%d" % (ti, h)
                        tap(C, "ssb" + x_, ssb[:, :], [128, 640], [ssb])
                        tap(C, "sm" + x_, sm[:, :], [128, 8], [sm])
                        tap(C, "pb" + x_, pb[:, :], [128, 640], [pb], BF16)
                        tap(C, "pT" + x_, pT[:, :], [128, 640], [pT], BF16)
                        tap(C, "qT" + x_, qT[:, :, :], [128, 16, 128], [qT], BF16)
                        tap(C, "qtok" + x_, qtok[:, 0, :], [128, 2048], [qtok], BF16)
                        tap(C, "hbf" + x_, hbf[:, :, 0:128], [128, 16, 128], [hbf], BF16)
                for kc0 in range(0, KC, 8):
                    for kk in range(8):
                        k = kc0 + kk
                        P.op("pe", lambda e, k=k, kk=kk: e.transpose(C.psbf[:, kk * 128:(kk + 1) * 128], otok[:, k * 128:(k + 1) * 128], C.identb[:]),
                             r=[otok, C.identb], w=[C.psbf])
                    P.op("act", lambda e, kc0=kc0, bl=bl: e.activation(oT[:, kc0:kc0 + 8, bl * 128:(bl + 1) * 128], C.psbf[:, 0:1024].rearrange("p (k t) -> p k t", k=8), AF.Copy),
                         r=[C.psbf], w=[oT])
            if C.dbg and ti in (0, 1):
                tap(C, "otok_t%d" % ti, otok[:, :], [128, 2048], [otok], BF16)
                tap(C, "oT_t%d" % ti, oT[:, :, :], [128, 16, 512], [oT], BF16)
                if ti == 0:
                    tap(C, "kT", kT[:, :, 0:1024], [128, 4, 1024], [kT], BF16)
                    tap(C, "kTfull", kT[:, :, :], [128, 4, NT], [kT], BF16)
                    tap(C, "vall", vall[:, 0:8, :], [128, 8, 512], [vall], BF16)
            for mg in range(4):
                Wo = load_group(wov, mg * 512)
                for mm in range(4):
                    m = mg * 4 + mm
                    ps = B[m % 2]
                    for k in range(KC):
                        P.op("pe", lambda e, ps=ps, Wo=Wo, k=k, mm=mm, n=n: e.matmul(ps[:, :n], Wo[k // 4][:, k % 4, mm * 128:(mm + 1) * 128], oT[:, k, :n],
                                                                                start=(k == 0), stop=(k == KC - 1)), r=[Wo[k // 4], oT], w=[ps])
                    gt = gate_ap(C, 0, cs_)
                    P.op("dve", lambda e, ps=ps, m=m, gt=gt, n=n: e.scalar_tensor_tensor(xbuf[:, m, :n], ps[:, :n], gt[:, m:m + 1], xbuf[:, m, :n], ALU.mult, ALU.add),
                         r=[ps, xbuf, C.mod], w=[xbuf])
            store_x(C, xbuf, ti)
        barrier(P)
        P.es = old
    barrier(P)


_NC_CACHE = {}


def _consts():
    k = {}
    sel = np.zeros((32, NE * 128), np.float32)
    for e in range(NE):
        sel[e, e * 128:(e + 1) * 128] = 1.0
    k["k_sel"] = sel
    s = np.arange(CH)[:, None]
    t = np.arange(CH)[None, :]
    k["k_mask"] = np.concatenate([(s <= t), (s >= t)], axis=1).astype(np.float32)
    tt = np.arange(LSEQ)
    invf = (10000.0 ** (-np.arange(16, dtype=np.float32) / 16)).astype(np.float32)
    ang_r = (tt // 64).astype(np.float32)[:, None] * invf
    ang_c = (tt % 64).astype(np.float32)[:, None] * invf
    rope = np.stack([np.stack([np.cos(ang_r), np.cos(ang_c)], 1), np.stack([np.sin(ang_r), np.sin(ang_c)], 1)], 1)
    k["k_rope"] = np.ascontiguousarray(rope.reshape(LSEQ, 64).astype(np.float32))
    i_ = np.arange(128)[:, None]
    j_ = np.arange(128)[None, :]
    k["k_amask"] = np.concatenate([np.where(j_ >= i_, 0.0, -1.0e4), np.where(j_ <= i_, 0.0, -1.0e4)], axis=1).astype(np.float32)
    k["k_tau"] = np.ascontiguousarray(np.broadcast_to(np.arange(TBK, dtype=np.float32)[None, :], (128, TBK)))
    k["k_cum"] = np.zeros((64, 128), np.float32)
    k["k_selb"] = np.zeros((64, 6), np.float32)
    return k


def _fm(v):
    v = np.asarray(v, np.float32)
    lead = v.shape[:-1]
    return np.ascontiguousarray(np.moveaxis(v.reshape(lead + (KC, 128)), -1, 0))


def prep_s5(inp):
    f = np.float32
    m = {}
    def st(v):
        return v.reshape(2, 64, 2, 64).transpose(0, 2, 3, 1).reshape(2, 128, 64)
    a_re = st(inp["s5_a_re"][0]); a_im = st(inp["s5_a_im"][0])
    ldt = st(np.broadcast_to(inp["s5_log_dt"][0][:, :, None], (2, 128, 64)))
    m["s5_a"] = np.ascontiguousarray(np.stack([a_re, a_im, ldt], 0).astype(f))
    bp = np.zeros((2, 128, 64, 128), f)
    cp = np.zeros((2, 128, 64, 128), f)
    for ci, (bsrc, csrc) in enumerate(((inp["s5_b_re"][0], inp["s5_c_re"][0]), (inp["s5_b_im"][0], inp["s5_c_im"][0]))):
        for g2 in range(2):
            for j in range(64):
                g = 2 * j + g2
                col = 32 * (j % 4) + 16 * g2
                bp[ci, g2 * 64:(g2 + 1) * 64, j, col:col + 16] = bsrc[g]
                cp[ci, g2 * 64:(g2 + 1) * 64, j, col:col + 16] = csrc[g].T
    m["s5_bp"] = bp
    m["s5_cp"] = cp
    m["s5_d"] = np.ascontiguousarray(inp["s5_d"][0].reshape(KC, 128).T.astype(f))
    m["s5_w_glu"] = inp["s5_w_glu"]
    return m


def prep_shared(inp):
    f = np.float32
    m = {}
    m["ada_w"] = inp["ada_w"]
    m["ada_b"] = np.ascontiguousarray(inp["ada_b"].reshape(4, 96, 128).transpose(0, 2, 1).astype(f))
    m["gmix"] = np.ascontiguousarray(inp["norm_mix_g"].reshape(4, KC, 128).transpose(2, 0, 1).reshape(128, 4 * KC).astype(f))
    m["gffn"] = np.ascontiguousarray(inp["norm_ffn_g"].reshape(4, KC, 128).transpose(2, 0, 1).reshape(128, 4 * KC).astype(f))
    m["gfin"] = np.ascontiguousarray(inp["final_norm_g"].reshape(KC, 128).T.astype(f))
    m["hgrn_w_in"] = inp["hgrn_w_in"]
    m["hgrn_w_out"] = inp["hgrn_w_out"]
    m["hgrn_gn"] = inp["hgrn_gnorm_g"]
    m["hgrn_lb"] = np.ascontiguousarray(inp["hgrn_lb_logits"].reshape(4, 16, 128).transpose(2, 0, 1).astype(f))
    m.update(prep_s5(inp))
    m["attn_w_qkv"] = inp["attn_w_qkv"]
    m["attn_w_o"] = inp["attn_w_o"]
    m["attn_sink"] = inp["attn_sink"]
    m["wr"] = np.ascontiguousarray(np.concatenate([inp["moe_w_group"], inp["moe_w_expert"]], axis=-1).astype(f))
    m["br"] = np.ascontiguousarray(np.concatenate([inp["moe_b_group"], inp["moe_b_expert"]], axis=-1).astype(f))
    m["moe_w_gate_up"] = inp["moe_w_gate_up"]
    m["moe_w_down"] = inp["moe_w_down"]
    m.update(_consts())
    return m


def prep_core(inp, b):
    f = np.float32
    m = {}
    m["xT0"] = np.ascontiguousarray(np.concatenate([inp["ctx"][b].T, inp["x"][b].T], axis=1).astype(f))
    c2 = np.stack([inp["c"][b], inp["c_ctx"]], axis=-1)
    m["c2"] = np.ascontiguousarray(c2.reshape(KC, 128, 2).transpose(1, 0, 2).astype(f))
    return m


def prep_inputs(inp, b):
    m = prep_shared(inp)
    m.update(prep_core(inp, b))
    return m


def kernel(**inp):
    inp = {k: np.asarray(v) for k, v in inp.items()}
    if "nc" not in _NC_CACHE:
        _NC_CACHE["nc"] = build(4)
    nc = _NC_CACHE["nc"]
    shared = prep_shared(inp)
    in_maps = []
    for b in range(8):
        m = dict(shared)
        m.update(prep_core(inp, b))
        in_maps.append(m)
    res = run_bass_kernel_spmd(nc, in_maps, core_ids=list(range(8)))
    out = np.stack([np.ascontiguousarray(r["outT"].T) for r in res.results], axis=0)
    return out.astype(np.float32)
```

```python
import numpy as np
from contextlib import ExitStack
from concourse.bass_utils import run_bass_kernel_spmd
import concourse.bass as bass
import concourse.mybir as mybir

F32 = mybir.dt.float32
BF16 = mybir.dt.bfloat16
I32 = mybir.dt.int32
ALU = mybir.AluOpType
AF = mybir.ActivationFunctionType
AX = mybir.AxisListType


class Dep:
    __slots__ = ("w", "r")

    def __init__(self):
        self.w = None
        self.r = []


class Tile:
    def __init__(self, t, dep=None):
        self.t = t
        self.dep = dep or Dep()

    def __getitem__(self, k):
        return self.t[k]


def _dep(x):
    return x.dep if isinstance(x, Tile) else x


class Prog:
    ENG = ("pe", "act", "dve", "pool", "sp")
    NDMA = 12

    def __init__(self, nc, es):
        self.nc = nc
        self.es = es
        self.stream = {e: [] for e in self.ENG}
        self.cnt = {e: 0 for e in self.ENG}
        self.sems = {}
        for e in self.ENG:
            self.sems[e] = es.enter_context(nc.semaphore("s_" + e))
        self.dma_sems = {}
        self.dma_cnt = {}
        self.dma_rr = {}
        for q in ("sp", "act", "pool"):
            self.dma_rr[q] = 0
            for i in range(self.NDMA):
                k = "d_%s_%d" % (q, i)
                self.sems[k] = es.enter_context(nc.semaphore(k))
                self.dma_cnt[k] = 0
        self.seen = {e: {} for e in self.ENG}
        self.ntile = 0
        self.ninstr = 0

    def sb(self, shape, dtype=F32, name=None):
        self.ntile += 1
        name = name or ("t%d" % self.ntile)
        t = self.es.enter_context(self.nc.sbuf_tensor(name, list(shape), dtype))
        return Tile(t)

    def ps(self, shape, dtype=F32, name=None):
        self.ntile += 1
        name = name or ("p%d" % self.ntile)
        t = self.es.enter_context(self.nc.psum_tensor(name, list(shape), dtype))
        return Tile(t)

    def dram(self, name, shape, dtype=F32, kind="Internal"):
        t = self.nc.dram_tensor(name, list(shape), dtype, kind=kind)
        return Tile(t.ap() if hasattr(t, "ap") else t)

    def _wait(self, eng, tok):
        key, val, src = tok
        if src == eng and eng == "pe":
            return
        if self.seen[eng].get(key, 0) >= val:
            return
        self.seen[eng][key] = val
        self.stream[eng].append(("w", key, val))

    def _deps(self, eng, r, w):
        for d in r:
            d = _dep(d)
            if d.w is not None:
                self._wait(eng, d.w)
        for d in w:
            d = _dep(d)
            if d.w is not None and d.w[2] != eng:
                self._wait(eng, d.w)
            elif d.w is not None and d.w[2] == eng and d.w[0].startswith("d_"):
                self._wait(eng, d.w)
            for t in d.r:
                if t[2] != eng or t[0].startswith("d_"):
                    self._wait(eng, t)

    def _mark(self, tok, r, w):
        for d in r:
            d = _dep(d)
            d.r.append(tok)
            if len(d.r) > 6:
                best = {}
                for t in d.r:
                    if t[0] not in best or best[t[0]][1] < t[1]:
                        best[t[0]] = t
                d.r = list(best.values())
        for d in w:
            d = _dep(d)
            d.w = tok
            d.r = []

    def op(self, eng, fn, r=(), w=()):
        self._deps(eng, r, w)
        self.cnt[eng] += 1
        tok = (eng, self.cnt[eng], eng)
        self.stream[eng].append(("o", fn, eng, 1))
        self._mark(tok, r, w)
        self.ninstr += 1
        return tok

    def dma(self, out, in_, r=(), w=(), q="sp", **kw):
        i = self.dma_rr[q]
        self.dma_rr[q] = (i + 1) % self.NDMA
        key = "d_%s_%d" % (q, i)
        if self.dma_cnt[key] > 0:
            self._wait(q, (key, 16 * self.dma_cnt[key], "dma"))
        self._deps(q, r, w)
        self.dma_cnt[key] += 1
        tok = (key, 16 * self.dma_cnt[key], "dma")
        self.stream[q].append(("o", lambda e: e.dma_start(out=out, in_=in_, **kw), key, 16))
        self._mark(tok, r, w)
        self.ninstr += 1
        return tok

    def wait_all(self, eng, toks):
        for t in toks:
            self._wait(eng, t)

    def emit(self):
        nc = self.nc
        sems = self.sems
        streams = self.stream

        def replay(name):
            def f(e):
                for it in streams[name]:
                    if it[0] == "w":
                        e.wait_ge(sems[it[1]], it[2])
                    else:
                        ins = it[1](e)
                        ins.then_inc(sems[it[2]], it[3])
            return f

        with nc.Block() as block:
            block.tensor(replay("pe"))
            block.scalar(replay("act"))
            block.vector(replay("dve"))
            block.gpsimd(replay("pool"))
            block.sync(replay("sp"))


D = 2048
KC = 16
NCTX = 256
LSEQ = 4096
NT = NCTX + LSEQ
EPS = 1e-6
TILES = [(0, 256)] + [(256 + 512 * i, 512) for i in range(8)]
NE = 32
DFF = 512


class Ctx:
    pass


TAPS = {}


def tap(C, name, ap, shape, deps, dt=F32):
    if not C.dbg or name in TAPS:
        return
    d = C.nc.dram_tensor("tap_" + name, list(shape), dt, kind="ExternalOutput").ap()
    TAPS[name] = d
    C.P.dma(d, ap, r=deps, w=[Tile(d)])


def barrier(P):
    toks = [(e, P.cnt[e], e) for e in P.ENG if P.cnt[e] > 0]
    toks += [(k, 16 * c, "dma") for k, c in P.dma_cnt.items() if c > 0]
    for e in P.ENG:
        for t in toks:
            P._wait(e, t)


def build(n_layers=4, dbg=False, stop=None, nlw=4, layers=None):
    nc = bass.Bass("TRN2", target_bir_lowering=False)
    es = ExitStack()
    P = Prog(nc, es)
    C = Ctx()
    C.nc, C.P = nc, P

    def din(name, shape, dt=F32):
        return Tile(nc.dram_tensor(name, list(shape), dt, kind="ExternalInput").ap())

    C.xT0 = din("xT0", [D, NT])
    C.c2 = din("c2", [128, KC, 2])
    C.ada_w = din("ada_w", [4, D, 6 * D])
    C.ada_b = din("ada_b", [4, 128, 96])
    C.gmix = din("gmix", [128, 4 * KC])
    C.gffn = din("gffn", [128, 4 * KC])
    C.gfin = din("gfin", [128, KC])
    C.hg_w_in = din("hgrn_w_in", [2, D, 5 * D])
    C.hg_w_out = din("hgrn_w_out", [2, D, D])
    C.hg_gn = din("hgrn_gn", [2, 128])
    C.hg_lb = din("hgrn_lb", [128, 4, 16])
    C.wr = din("wr", [4, D, 36])
    C.br = din("br", [4, 36])
    C.w_gu = din("moe_w_gate_up", [4, NE, D, 2 * DFF])
    C.w_dn = din("moe_w_down", [4, NE, DFF, D])
    C.s5_a = din("s5_a", [3, 2, 128, 64])
    C.s5_bp = din("s5_bp", [2, 128, 64, 128])
    C.s5_cp = din("s5_cp", [2, 128, 64, 128])
    C.s5_d = din("s5_d", [128, KC])
    C.s5_wglu = din("s5_w_glu", [1, D, 2 * D])
    C.k_tau = din("k_tau", [128, TBK])
    C.w_qkv = din("attn_w_qkv", [1, D, 3072])
    C.w_o = din("attn_w_o", [1, D, D])
    C.sink = din("attn_sink", [1, 32])
    C.k_rope = din("k_rope", [LSEQ, 64])
    C.k_amask = din("k_amask", [128, 256])
    C.k_sel = din("k_sel", [32, NE * 128])
    C.k_cum = din("k_cum", [64, 2 * 64])
    C.k_selb = din("k_selb", [64, 2 * 3])
    C.k_mask = din("k_mask", [CH, 2 * CH])
    C.outT = Tile(nc.dram_tensor("outT", [D, LSEQ], F32, kind="ExternalOutput").ap())
    C.XT = Tile(nc.dram_tensor("XT", [D, NT], F32, kind="Internal").ap())
    C.OF = Tile(nc.dram_tensor("OF", [D, NT], F32, kind="Internal").ap())
    C.OFT = C.OF.t
    C.dbg = dbg
    C.tapdeps = []
    C.XTv = C.XT.t.rearrange("(k p) t -> p k t", p=128)
    C.xT0v = C.xT0.t.rearrange("(k p) t -> p k t", p=128)
    C.outTv = C.outT.t.rearrange("(k p) t -> p k t", p=128)
    C.xdep = [Dep() for _ in TILES]

    C.identf = P.sb([128, 128], F32, "identf")
    C.identb = P.sb([128, 128], BF16, "identb")
    C.ones = P.sb([128, 128], F32, "ones")
    C.epsb = P.sb([128, 1], F32, "epsb")
    C.sc2 = P.sb([128, KC, 2], F32, "sc2")
    C.mod = P.sb([128, 96, 2], F32, "mod")
    C.adab = P.sb([128, 96], F32, "adab")
    C.gmix_s = P.sb([128, 4 * KC], F32, "gmix_s")
    C.gffn_s = P.sb([128, 4 * KC], F32, "gffn_s")
    C.gfin_s = P.sb([128, KC], F32, "gfin_s")
    C.AB = P.sb([128, 4, KC, 2], F32, "AB")
    C.AB2 = P.sb([128, 4, KC], F32, "AB2")
    C.psb = [P.ps([128, 512], F32, "bank%d" % i) for i in range(7)]
    C.psbf = P.ps([128, 1024], BF16, "bankbf")

    P.op("pool", lambda e: e.memset(C.identf[:], 1.0), w=[C.identf])
    P.op("pool", lambda e: e.affine_select(C.identf[:], C.identf[:], [[-1, 128]], ALU.is_equal, 0.0,
                                           base=0, channel_multiplier=1), r=[C.identf], w=[C.identf])
    P.op("dve", lambda e: e.tensor_copy(C.identb[:], C.identf[:]), r=[C.identf], w=[C.identb])
    P.op("dve", lambda e: e.memset(C.ones[:], 1.0), w=[C.ones])
    P.op("dve", lambda e: e.memset(C.epsb[:], EPS), w=[C.epsb])
    P.dma(C.sc2[:], C.c2.t[:, :, :], r=[C.c2], w=[C.sc2])
    P.op("act", lambda e: e.activation(C.sc2[:], C.sc2[:], AF.Silu), r=[C.sc2], w=[C.sc2])
    P.dma(C.gmix_s[:], C.gmix.t[:, :], w=[C.gmix_s])
    P.dma(C.gffn_s[:], C.gffn.t[:, :], w=[C.gffn_s])
    P.dma(C.gfin_s[:], C.gfin.t[:, :], w=[C.gfin_s])
    for ti, (t0, n) in enumerate(TILES):
        P.dma(C.XT.t[:, t0:t0 + n], C.xT0.t[:, t0:t0 + n], r=[C.xT0], w=[C.xdep[ti]])

    C.stop = stop
    for li in (layers if layers is not None else range(n_layers)):
        ada_phase(C, li)
        kind = li % 3
        if kind == 0:
            hgrn_phase(C, li, li // 3, with_ctx=(li < 3))
        elif kind == 1:
            s5_phase(C, li, with_ctx=(li < 3))
        else:
            attn_phase(C, li, with_ctx=(li < 3))
        if dbg:
            d_ = Tile(nc.dram_tensor("xmix%d" % li, [D, NT], F32, kind="ExternalOutput").ap())
            P.dma(d_.t[:, :], C.XT.t[:, :], r=C.xdep, w=[d_])
            barrier(P)
        if stop != "mix":
            moe_phase(C, li, with_ctx=(li < 3))
        if dbg:
            d_ = Tile(nc.dram_tensor("xdbg%d" % li, [D, NT], F32, kind="ExternalOutput").ap())
            P.dma(d_.t[:, :], C.XT.t[:, :], r=C.xdep, w=[d_])
            barrier(P)
    final_phase(C, n_layers)
    for q in ("sp", "pool", "act"):
        for i in range(P.NDMA):
            k = "d_%s_%d" % (q, i)
            if P.dma_cnt[k]:
                P._wait("sp", (k, 16 * P.dma_cnt[k], "dma"))
    P.emit()
    es.close()
    return nc


def ada_phase(C, li):
    P = C.P
    with ExitStack() as es2:
        old = P.es
        P.es = es2
        wb = [P.sb([128, KC, 512], F32, "adaw%d_%d" % (li, i)) for i in range(2)]
        P.dma(C.adab[:], C.ada_b.t[li, :, :], w=[C.adab])
        wv = C.ada_w.t[li].rearrange("(k p) n -> p k n", p=128)
        ps = C.psb[0]
        for g in range(24):
            w = wb[g % 2]
            for h in range(2):
                P.dma(w[:, h * 8:(h + 1) * 8, :], wv[:, h * 8:(h + 1) * 8, g * 512:(g + 1) * 512], w=[w],
                      q=("sp" if h == 0 else "act"))
            for jj in range(4):
                j = g * 4 + jj
                for k in range(KC):
                    P.op("pe", lambda e, w=w, jj=jj, k=k, j=j: e.matmul(
                        ps[:, j * 2:(j + 1) * 2], w[:, k, jj * 128:(jj + 1) * 128], C.sc2[:, k, :],
                        start=(k == 0), stop=(k == KC - 1)), r=[w, C.sc2], w=[ps])
        P.op("dve", lambda e: e.tensor_tensor(
            C.mod[:], ps[:, 0:192].rearrange("p (j c) -> p j c", c=2),
            C.adab[:].unsqueeze(2).to_broadcast([128, 96, 2]), ALU.add), r=[ps, C.adab], w=[C.mod])
        for ni in range(4):
            c = ni % 2
            base = 0 if ni < 2 else 3
            g = (C.gmix_s if ni < 2 else C.gffn_s)
            sh = C.mod[:, (base + 0) * 16:(base + 1) * 16, c]
            sc = C.mod[:, (base + 1) * 16:(base + 2) * 16, c]
            P.op("dve", lambda e, ni=ni, sc=sc, g=g: e.scalar_tensor_tensor(
                C.AB[:, ni, :, 0], sc, 1.0, g[:, li * 16:(li + 1) * 16], ALU.add, ALU.mult),
                r=[C.mod, g], w=[C.AB])
            P.op("dve", lambda e, ni=ni, sh=sh: e.tensor_copy(C.AB[:, ni, :, 1], sh), r=[C.mod], w=[C.AB])
        barrier(P)
        P.es = old
    barrier(P)


def gate_ap(C, which, c):
    base = 2 if which == 0 else 5
    return C.mod[:, base * 16:(base + 1) * 16, c]


def load_x(C, xbuf, ti, q="sp"):
    t0, n = TILES[ti]
    P = C.P
    for h in range(2):
        P.dma(xbuf[:, h * 8:(h + 1) * 8, :n], C.XTv[:, h * 8:(h + 1) * 8, t0:t0 + n], r=[C.xdep[ti]], w=[xbuf],
              q=("sp" if h == 0 else "act"))


def store_x(C, xbuf, ti):
    t0, n = TILES[ti]
    P = C.P
    for h in range(2):
        P.dma(C.XTv[:, h * 8:(h + 1) * 8, t0:t0 + n], xbuf[:, h * 8:(h + 1) * 8, :n], r=[xbuf], w=[C.xdep[ti]],
              q="sp")


def norm_mod(C, xbuf, n, ni, hbf, tmp, rstd, h32=None, psbank=7):
    P = C.P
    ps = C.psb[psbank]
    for k in range(KC):
        t = tmp[k % len(tmp)]
        P.op("act", lambda e, t=t, k=k: e.activation(t[:, :n], xbuf[:, k, :n], AF.Square), r=[xbuf], w=[t])
        P.op("pe", lambda e, t=t, k=k: e.matmul(ps[:, :n], C.ones[:], t[:, :n], start=(k == 0), stop=(k == KC - 1)),
             r=[t, C.ones], w=[ps])
    P.op("act", lambda e: e.activation(rstd[:, :n], ps[:, :n], AF.Sqrt, bias=C.epsb[:, 0:1], scale=1.0 / D),
         r=[ps, C.epsb], w=[rstd])
    P.op("dve", lambda e: e.reciprocal(rstd[:, :n], rstd[:, :n]), r=[rstd], w=[rstd])
    for k in range(KC):
        t = tmp[k % len(tmp)]
        P.op("dve", lambda e, t=t, k=k: e.tensor_tensor(t[:, :n], xbuf[:, k, :n], rstd[:, :n], ALU.mult),
             r=[xbuf, rstd], w=[t])
        if h32 is not None:
            P.op("act", lambda e, t=t, k=k: e.activation(h32[:, k, :n], t[:, :n], AF.Identity,
                                                         bias=C.AB[:, ni, k, 1:2], scale=C.AB[:, ni, k, 0:1]),
                 r=[t, C.AB], w=[h32])
            P.op("pool", lambda e, k=k: e.tensor_copy(hbf[:, k, :n], h32[:, k, :n]), r=[h32], w=[hbf])
        else:
            P.op("act", lambda e, t=t, k=k: e.activation(hbf[:, k, :n], t[:, :n], AF.Identity,
                                                         bias=C.AB[:, ni, k, 1:2], scale=C.AB[:, ni, k, 0:1]),
                 r=[t, C.AB], w=[hbf])


def hgrn_phase(C, li, j, with_ctx):
    P = C.P
    with ExitStack() as es2:
        old = P.es
        P.es = es2
        S = Ctx()
        S.xbuf = P.sb([128, KC, 512], F32)
        S.hbf = P.sb([128, KC, 512], BF16)
        S.tmp = [P.sb([128, 512], F32) for _ in range(2)]
        S.rstd = P.sb([128, 512], F32)
        S.ring = [[P.sb([128, 4, 512], BF16) for _ in range(4)] for _ in range(4)]
        S.state = P.sb([128, 16, 128], F32)
        S.rst = P.sb([128, 512], F32)
        S.mask = P.sb([CH, 2, CH], F32)
        S.lbt = P.sb([128, 4, 16], F32)
        S.lb = P.sb([128, 16], F32)
        S.keep = P.sb([128, 16], F32)
        S.lbw = P.sb([128, 16], F32)
        S.gn = P.sb([128, 1], F32)
        S.oT = P.sb([128, KC, 512], BF16)
        S.qT = [P.sb([128, 512], F32) for _ in range(2)]
        S.vT = [P.sb([128, 512], BF16) for _ in range(2)]
        S.sg = [P.sb([128, 512], F32)] * 2
        S.g = [P.sb([128, 512], F32)] * 2
        S.bb = [P.sb([128, 512], F32)] * 2
        S.bp = [P.sb([128, 512], F32)] * 2
        S.ep = [P.sb([128, 512], F32)] * 2
        S.en = [P.sb([128, 512], F32)] * 2
        S.qt = [P.sb([128, 512], BF16) for _ in range(2)]
        S.kt = [P.sb([128, 512], BF16) for _ in range(2)]
        S.E = [P.sb([128, 3, 32], F32) for _ in range(2)]
        S.ktok = [P.sb([CH, 4096], BF16)] * 2
        S.vtok = [P.sb([CH, 4096], BF16)] * 2
        S.sT = [P.sb([CH, 512], BF16) for _ in range(2)]
        S.kvs = [P.sb([128, 4, 128], F32) for _ in range(2)]
        S.stb = [P.sb([128, 128], BF16) for _ in range(2)]
        S.of = [P.sb([128, 512], F32)] * 2
        S.o = [P.sb([128, 512], F32)] * 2
        S.sog = [P.sb([128, 512], F32)] * 2
        S.ofdep = [[Dep() for _ in range(16)] for _ in TILES]
        S.sdep = [Dep() for _ in range(16)]
        S.oTdep = [Dep() for _ in range(16)]

        P.op("dve", lambda e: e.memset(S.rst[:], 1.0), w=[S.rst])
        P.op("dve", lambda e: e.memset(S.rst[:].rearrange("p (u t) -> p u t", t=CH)[:, :, 0:1], 0.0), w=[S.rst])
        P.dma(S.mask[:], C.k_mask.t[:, :].rearrange("p (a b) -> p a b", a=2), w=[S.mask])
        P.dma(S.gn[:], C.hg_gn.t[j].rearrange("(p o) -> p o", o=1), w=[S.gn])
        P.dma(S.lbt[:], C.hg_lb.t[:, :, :], w=[S.lbt])
        P.op("dve", lambda e: e.tensor_reduce(S.lbw[:], S.lbt[:].rearrange("p l h -> p h l"), AX.X, ALU.max), r=[S.lbt], w=[S.lbw])
        P.op("dve", lambda e: e.tensor_tensor(S.lbt[:], S.lbt[:], S.lbw[:].unsqueeze(1).to_broadcast([128, 4, 16]), ALU.subtract),
             r=[S.lbt, S.lbw], w=[S.lbt])
        P.op("act", lambda e: e.activation(S.lbt[:], S.lbt[:], AF.Exp), r=[S.lbt], w=[S.lbt])
        P.op("dve", lambda e: e.tensor_reduce(S.lbw[:], S.lbt[:].rearrange("p l h -> p h l"), AX.X, ALU.add), r=[S.lbt], w=[S.lbw])
        P.op("dve", lambda e: e.reciprocal(S.lbw[:], S.lbw[:]), r=[S.lbw], w=[S.lbw])
        if li == 0:
            P.op("dve", lambda e: e.memset(S.lb[:], 0.0), w=[S.lb])
        else:
            P.op("dve", lambda e: e.tensor_reduce(S.lb[:], S.lbt[:, 1:li + 1, :].rearrange("p l h -> p h l"), AX.X, ALU.add),
                 r=[S.lbt], w=[S.lb])
            P.op("dve", lambda e: e.tensor_tensor(S.lb[:], S.lb[:], S.lbw[:], ALU.mult), r=[S.lb, S.lbw], w=[S.lb])
        P.op("dve", lambda e: e.tensor_scalar(S.keep[:], S.lb[:], -1.0, 1.0, ALU.mult, ALU.add), r=[S.lb], w=[S.keep])

        win = C.hg_w_in.t[j].rearrange("(k p) n -> p k n", p=128)
        wout = C.hg_w_out.t[j].rearrange("(k p) n -> p k n", p=128)
        ringpos = [0]

        def load_group(src, col0):
            buf = S.ring[ringpos[0] % 4]
            ringpos[0] += 1
            for q4 in range(4):
                P.dma(buf[q4][:], src[:, q4 * 4:(q4 + 1) * 4, col0:col0 + 512], w=[buf[q4]], q="pool")
            return buf

        for pas in (0, 1):
            order = list(range(9)) if pas == 0 else [0] + list(range(8, 0, -1))
            types = [0, 1, 2] if pas == 0 else [0, 1, 3, 4]
            P.op("dve", lambda e: e.memset(S.state[:], 0.0), w=S.sdep)
            for ti in order:
                t0, n = TILES[ti]
                cs = 1 if ti == 0 else 0
                load_x(C, S.xbuf, ti)
                norm_mod(C, S.xbuf, n, cs, S.hbf, S.tmp, S.rstd, psbank=0)
                groups = None
                for hg in range(4):
                    W = [load_group(win, ty * 2048 + hg * 512) for ty in types]
                    for hh in range(4):
                        hgrn_head(C, S, li, pas, ti, hg * 4 + hh, hh, W)
                if pas == 1:
                    load_x(C, S.xbuf, ti)
                    for mg in range(4):
                        Wo = load_group(wout, mg * 512)
                        for mm in range(4):
                            m = mg * 4 + mm
                            ps = C.psb[m % 2]
                            for k in range(KC):
                                P.op("pe", lambda e, ps=ps, Wo=Wo, k=k, mm=mm, n=n: e.matmul(
                                    ps[:, :n], Wo[k // 4][:, k % 4, mm * 128:(mm + 1) * 128], S.oT[:, k, :n],
                                    start=(k == 0), stop=(k == KC - 1)), r=[Wo[k // 4]] + S.oTdep, w=[ps])
                            gt = gate_ap(C, 0, cs)
                            P.op("dve", lambda e, ps=ps, m=m, gt=gt, n=n: e.scalar_tensor_tensor(
                                S.xbuf[:, m, :n], ps[:, :n], gt[:, m:m + 1], S.xbuf[:, m, :n], ALU.mult, ALU.add),
                                r=[ps, S.xbuf, C.mod], w=[S.xbuf])
                    store_x(C, S.xbuf, ti)
        barrier(P)
        P.es = old
    barrier(P)


CH = 16


def hgrn_head(C, S, li, pas, ti, h, hh, W):
    P = C.P
    t0, n = TILES[ti]
    nu = n // CH
    b = h % 2
    B = C.psb
    ref = CH // 2 - 1 if pas == 0 else CH // 2
    last = CH - 1 if pas == 0 else 0
    OFv = C.OFT

    def proj(ps, Wp):
        for k in range(KC):
            P.op("pe", lambda e, k=k: e.matmul(ps[:, :n], Wp[k // 4][:, k % 4, hh * 128:(hh + 1) * 128], S.hbf[:, k, :n],
                                               start=(k == 0), stop=(k == KC - 1)), r=[Wp[k // 4], S.hbf], w=[ps])

    if pas == 1:
        P.dma(S.of[b][:, :n], OFv[h * 128:(h + 1) * 128, t0:t0 + n], r=[S.ofdep[ti][h]], w=[S.of[b]])
    proj(B[0], W[0])
    proj(B[1], W[1])
    proj(B[2], W[2])
    if pas == 1:
        proj(B[3], W[3])
    qT, vT, sg, g, bb, bp, ep, en, qt, kt, E = S.qT[b], S.vT[b], S.sg[b], S.g[b], S.bb[b], S.bp[b], S.ep[b], S.en[b], S.qt[b], S.kt[b], S.E[b]
    P.op("act", lambda e: e.activation(qT[:, :n], B[0][:, :n], AF.Silu), r=[B[0]], w=[qT])
    P.op("dve", lambda e: e.tensor_copy(vT[:, :n], B[1][:, :n]), r=[B[1]], w=[vT])
    P.op("act", lambda e: e.activation(sg[:, :n], B[2][:, :n], AF.Sigmoid), r=[B[2]], w=[sg])
    P.op("dve", lambda e: e.tensor_scalar(sg[:, :n], sg[:, :n], S.keep[:, h:h + 1], S.lb[:, h:h + 1], ALU.mult, ALU.add),
         r=[sg, S.keep, S.lb], w=[sg])
    P.op("act", lambda e: e.activation(g[:, :n], sg[:, :n], AF.Ln), r=[sg], w=[g])
    P.op("dve", lambda e: e.tensor_scalar(sg[:, :n], sg[:, :n], -1.0, 1.0, ALU.mult, ALU.add), r=[sg], w=[sg])
    P.op("dve", lambda e: e.tensor_tensor_scan(bb[:, :n], S.rst[:, :n], g[:, :n], 0.0, ALU.mult, ALU.add),
         r=[S.rst, g], w=[bb])
    v3 = lambda t: t[:, :n].rearrange("p (u t) -> p u t", t=CH)
    if pas == 1:
        P.op("dve", lambda e: e.tensor_tensor(g[:, :n], g[:, :n], bb[:, :n], ALU.subtract), r=[g, bb], w=[g])
        P.op("dve", lambda e: e.tensor_tensor(v3(g), v3(g), v3(bb)[:, :, CH - 1:CH].to_broadcast([128, nu, CH]), ALU.add),
             r=[g, bb], w=[g])
        bsrc = g
    else:
        bsrc = bb
    P.op("dve", lambda e: e.tensor_tensor(v3(bp), v3(bsrc), v3(bsrc)[:, :, ref:ref + 1].to_broadcast([128, nu, CH]), ALU.subtract),
         r=[bsrc], w=[bp])
    P.op("act", lambda e: e.activation(ep[:, :n], bp[:, :n], AF.Exp), r=[bp], w=[ep])
    P.op("act", lambda e: e.activation(en[:, :n], bp[:, :n], AF.Exp, scale=-1.0), r=[bp], w=[en])
    P.op("act", lambda e: e.activation(E[:, 0, :nu], v3(bsrc)[:, :, ref], AF.Exp), r=[bsrc], w=[E])
    P.op("act", lambda e: e.activation(E[:, 1, :nu], v3(bsrc)[:, :, last], AF.Exp), r=[bsrc], w=[E])
    P.op("act", lambda e: e.activation(E[:, 2, :nu], v3(bp)[:, :, last], AF.Exp), r=[bp], w=[E])
    P.op("dve", lambda e: e.scalar_tensor_tensor(qt[:, :n], qT[:, :n], 128.0 ** -0.5, ep[:, :n], ALU.mult, ALU.mult),
         r=[qT, ep], w=[qt])
    P.op("dve", lambda e: e.tensor_tensor(kt[:, :n], sg[:, :n], en[:, :n], ALU.mult), r=[sg, en], w=[kt])
    ktok, vtok, sT = S.ktok[b], S.vtok[b], S.sT[b]
    for (src, dst) in ((kt, ktok), (vT, vtok)):
        for u0 in range(0, nu, 8):
            for u in range(u0, u0 + 8):
                P.op("pe", lambda e, u=u, src=src: e.transpose(C.psbf[0:CH, (u % 8) * 128:(u % 8 + 1) * 128], src[:, u * CH:(u + 1) * CH], C.identb[:]),
                     r=[src, C.identb], w=[C.psbf])
            P.op("act", lambda e, dst=dst, u0=u0: e.activation(dst[:, u0 * 128:(u0 + 8) * 128], C.psbf[0:CH, 0:1024], AF.Copy), r=[C.psbf], w=[dst])
    for u in range(nu):
        P.op("pe", lambda e, u=u: e.matmul(B[4][0:CH, u * CH:(u + 1) * CH], kt[:, u * CH:(u + 1) * CH], qt[:, u * CH:(u + 1) * CH],
                                           start=True, stop=True), r=[kt, qt], w=[B[4]])
    P.op("dve", lambda e: e.tensor_tensor(sT[:, :n].rearrange("p (u t) -> p u t", t=CH),
                                          B[4][0:CH, :n].rearrange("p (u t) -> p u t", t=CH),
                                          S.mask[:, pas, :].unsqueeze(1).to_broadcast([CH, nu, CH]), ALU.mult),
         r=[B[4], S.mask], w=[sT])
    st = S.state
    ngr = nu // 4
    grs = list(range(ngr)) if pas == 0 else list(range(ngr - 1, -1, -1))
    for gi, gr in enumerate(grs):
        kvs = S.kvs[gi % 2]
        for uu in range(4):
            u = gr * 4 + uu
            P.op("pe", lambda e, u=u, uu=uu: e.matmul(B[6][:, uu * 128:(uu + 1) * 128], ktok[:, u * 128:(u + 1) * 128],
                                                      vtok[:, u * 128:(u + 1) * 128], start=True, stop=True), r=[ktok, vtok], w=[B[6]])
        P.op("dve", lambda e, gr=gr, kvs=kvs: e.tensor_tensor(kvs[:], B[6][:, :].rearrange("p (u v) -> p u v", v=128),
                                                              E[:, 2, gr * 4:gr * 4 + 4].unsqueeze(2).to_broadcast([128, 4, 128]), ALU.mult),
             r=[B[6], E], w=[kvs])
        us = list(range(4)) if pas == 0 else [3, 2, 1, 0]
        for i, uu in enumerate(us):
            u = gr * 4 + uu
            sb_ = S.stb[i % 2]
            P.op("dve", lambda e, u=u, sb_=sb_: e.tensor_scalar(sb_[:], st[:, h, :], E[:, 0, u:u + 1], None, ALU.mult),
                 r=[S.sdep[h], E], w=[sb_])
            P.op("pe", lambda e, u=u: e.matmul(B[5][:, u * CH:(u + 1) * CH], vtok[:, u * 128:(u + 1) * 128], sT[:, u * CH:(u + 1) * CH],
                                               start=True, stop=False), r=[vtok, sT], w=[B[5]])
            P.op("pe", lambda e, u=u, sb_=sb_: e.matmul(B[5][:, u * CH:(u + 1) * CH], sb_[:], qt[:, u * CH:(u + 1) * CH],
                                                        start=False, stop=True), r=[sb_, qt], w=[B[5]])
            P.op("dve", lambda e, u=u, uu=uu, kvs=kvs: e.scalar_tensor_tensor(st[:, h, :], st[:, h, :], E[:, 1, u:u + 1], kvs[:, uu, :], ALU.mult, ALU.add),
                 r=[S.sdep[h], E, kvs], w=[S.sdep[h]])
    if pas == 0:
        P.op("act", lambda e: e.activation(S.o[b][:, :n], B[5][:, :n], AF.Copy), r=[B[5]], w=[S.o[b]])
        P.dma(OFv[h * 128:(h + 1) * 128, t0:t0 + n], S.o[b][:, :n], r=[S.o[b]], w=[S.ofdep[ti][h]])
    else:
        o, sog = S.o[b], S.sog[b]
        P.op("dve", lambda e: e.tensor_tensor(o[:, :n], B[5][:, :n], S.of[b][:, :n], ALU.add), r=[B[5], S.of[b]], w=[o])
        P.op("act", lambda e: e.activation(ep[:, :n], o[:, :n], AF.Square), r=[o], w=[ep])
        P.op("pe", lambda e: e.matmul(B[4][:, :n], C.ones[:], ep[:, :n], start=True, stop=True), r=[ep, C.ones], w=[B[4]])
        P.op("act", lambda e: e.activation(en[:, :n], B[4][:, :n], AF.Sqrt, bias=C.epsb[:, 0:1], scale=1.0 / 128), r=[B[4], C.epsb], w=[en])
        P.op("dve", lambda e: e.reciprocal(en[:, :n], en[:, :n]), r=[en], w=[en])
        P.op("act", lambda e: e.activation(sog[:, :n], B[3][:, :n], AF.Silu), r=[B[3]], w=[sog])
        P.op("dve", lambda e: e.tensor_tensor(o[:, :n], o[:, :n], en[:, :n], ALU.mult), r=[o, en], w=[o])
        P.op("dve", lambda e: e.scalar_tensor_tensor(S.oT[:, h, :n], o[:, :n], S.gn[:, 0:1], sog[:, :n], ALU.mult, ALU.mult),
             r=[o, S.gn, sog], w=[S.oTdep[h]])


def moe_phase(C, li, with_ctx):
    P = C.P
    B = C.psb
    tiles = list(range(9)) if with_ctx else list(range(1, 9))
    with ExitStack() as es2:
        old = P.es
        P.es = es2
        buf = P.sb([128, KC, 512], F32)
        bd = [Dep() for _ in range(KC)]
        hbf = P.sb([128, KC, 512], BF16)
        wgu = [[P.sb([128, 4, 1024], BF16) for _ in range(4)] for _ in range(2)]
        wdn = [[P.sb([128, 2, 2048], BF16) for _ in range(2)] for _ in range(2)]
        u = [[P.sb([128, 512], BF16) for _ in range(4)] for _ in range(2)]
        tS = [P.sb([128, 512], F32) for _ in range(2)]
        tT = [P.sb([128, 512], F32) for _ in range(2)]
        rstd = P.sb([128, 512], F32)
        wr_s = P.sb([128, KC, 36], F32)
        br_s = P.sb([128, 36], F32)
        gatesT = P.sb([32, 512], F32)
        gm = [P.sb([32, 512], F32) for _ in range(2)]
        rt = P.sb([128, 192], F32)
        xs = [P.sb([128, 4, 512], F32) for _ in range(2)]
        P.dma(wr_s[:], C.wr.t[li].rearrange("(k p) n -> p k n", p=128), w=[wr_s])
        P.dma(br_s[:], C.br.t[li:li + 1, :].partition_broadcast(128), w=[br_s])
        seq = [(ti, e) for ti in tiles for e in range(NE)]

        def issue_w(idx):
            ti, e = seq[idx]
            b = idx % 2
            gv = C.w_gu.t[li, e].rearrange("(k p) n -> p k n", p=128)
            dv = C.w_dn.t[li, e].rearrange("(j p) n -> p j n", p=128)
            for q4 in range(4):
                P.dma(wgu[b][q4][:], gv[:, q4 * 4:(q4 + 1) * 4, :], w=[wgu[b][q4]], q="pool")
            for q2 in range(2):
                P.dma(wdn[b][q2][:], dv[:, q2 * 2:(q2 + 1) * 2, :], w=[wdn[b][q2]], q="pool")

        def prologue(ti):
            t0, n = TILES[ti]
            cs = 1 if ti == 0 else 0
            ni = 3 if ti == 0 else 2
            for hlf in range(2):
                P.dma(buf[:, hlf * 8:(hlf + 1) * 8, :n], C.XTv[:, hlf * 8:(hlf + 1) * 8, t0:t0 + n], r=[C.xdep[ti]],
                      w=bd[hlf * 8:(hlf + 1) * 8])
            for k in range(KC):
                t = tS[k % 2]
                P.op("act", lambda e, t=t, k=k: e.activation(t[:, :n], buf[:, k, :n], AF.Square), r=[bd[k]], w=[t])
                P.op("pe", lambda e, t=t, k=k: e.matmul(B[0][:, :n], C.ones[:], t[:, :n], start=(k == 0), stop=(k == KC - 1)),
                     r=[t, C.ones], w=[B[0]])
            P.op("act", lambda e: e.activation(rstd[:, :n], B[0][:, :n], AF.Sqrt, bias=C.epsb[:, 0:1], scale=1.0 / D),
                 r=[B[0], C.epsb], w=[rstd])
            P.op("dve", lambda e: e.reciprocal(rstd[:, :n], rstd[:, :n]), r=[rstd], w=[rstd])
            for k in range(KC):
                P.op("dve", lambda e, k=k: e.tensor_tensor(buf[:, k, :n], buf[:, k, :n], rstd[:, :n], ALU.mult),
                     r=[bd[k], rstd], w=[bd[k]])
                P.op("act", lambda e, k=k: e.activation(buf[:, k, :n], buf[:, k, :n], AF.Identity,
                                                        bias=C.AB[:, ni, k, 1:2], scale=C.AB[:, ni, k, 0:1]),
                     r=[bd[k], C.AB], w=[bd[k]])
                P.op("dve", lambda e, k=k: e.tensor_copy(hbf[:, k, :n], buf[:, k, :n]), r=[bd[k]], w=[hbf])
            for s in range(n // 128):
                for k in range(KC):
                    P.op("pe", lambda e, k=k, s=s: e.matmul(B[1][:, 0:36], buf[:, k, s * 128:(s + 1) * 128], wr_s[:, k, :],
                                                            start=(k == 0), stop=(k == KC - 1)), r=[bd[k], wr_s], w=[B[1]])
                route(s)

        def route(s):
            R = lambda a, b_: rt[:, a:b_]
            lg, gmax, ngm, eg, gsum, psel = R(0, 36), R(36, 37), R(37, 38), R(38, 42), R(42, 43), R(43, 44)
            ohg, lsel, logg, m1, oh1 = R(44, 48), R(48, 80), R(80, 88), R(88, 89), R(89, 97)
            msk, m2, oh2, dd, e2, w1, w2 = R(97, 105), R(105, 106), R(106, 114), R(114, 115), R(115, 116), R(116, 117), R(117, 118)
            gin, gates = R(118, 126), R(128, 160)
            D1 = lambda fn, rr=(): P.op("dve", fn, r=[rt] + list(rr), w=[rt])
            A1 = lambda fn: P.op("act", fn, r=[rt], w=[rt])
            D1(lambda e: e.tensor_tensor(lg, B[1][:, 0:36], br_s[:], ALU.add), [B[1], br_s])
            D1(lambda e: e.reduce_max(gmax, lg[:, 0:4], AX.X))
            D1(lambda e: e.tensor_scalar(ngm, gmax, -1.0, None, ALU.mult))
            A1(lambda e: e.activation(eg, lg[:, 0:4], AF.Exp, bias=ngm, scale=1.0))
            D1(lambda e: e.reduce_sum(gsum, eg, AX.X))
            D1(lambda e: e.reciprocal(psel, gsum))
            D1(lambda e: e.tensor_scalar(ohg, lg[:, 0:4], gmax, None, ALU.is_equal))
            D1(lambda e: e.tensor_tensor(lsel.rearrange("p (g x) -> p g x", g=4), lg[:, 4:36].rearrange("p (g x) -> p g x", g=4),
                                         ohg.unsqueeze(2).to_broadcast([128, 4, 8]), ALU.mult))
            D1(lambda e: e.tensor_reduce(logg, lsel.rearrange("p (g x) -> p x g", g=4), AX.X, ALU.add))
            D1(lambda e: e.reduce_max(m1, logg, AX.X))
            D1(lambda e: e.tensor_scalar(oh1, logg, m1, None, ALU.is_equal))
            D1(lambda e: e.scalar_tensor_tensor(msk, oh1, -1e30, logg, ALU.mult, ALU.add))
            D1(lambda e: e.reduce_max(m2, msk, AX.X))
            D1(lambda e: e.tensor_scalar(oh2, msk, m2, None, ALU.is_equal))
            D1(lambda e: e.tensor_tensor(dd, m2, m1, ALU.subtract))
            A1(lambda e: e.activation(e2, dd, AF.Exp))
            D1(lambda e: e.tensor_scalar(w1, e2, 1.0, None, ALU.add))
            D1(lambda e: e.reciprocal(w1, w1))
            D1(lambda e: e.tensor_scalar(w2, w1, -1.0, 1.0, ALU.mult, ALU.add))
            D1(lambda e: e.tensor_tensor(w1, w1, psel, ALU.mult))
            D1(lambda e: e.tensor_tensor(w2, w2, psel, ALU.mult))
            D1(lambda e: e.tensor_scalar(gin, oh1, w1, None, ALU.mult))
            D1(lambda e: e.scalar_tensor_tensor(gin, oh2, w2, gin, ALU.mult, ALU.add))
            D1(lambda e: e.tensor_tensor(gates.rearrange("p (g x) -> p g x", g=4), ohg.unsqueeze(2).to_broadcast([128, 4, 8]),
                                         gin.unsqueeze(1).to_broadcast([128, 4, 8]), ALU.mult))
            P.op("pe", lambda e: e.transpose(B[2][0:32, 0:128], gates, C.identf[:]), r=[rt, C.identf], w=[B[2]])
            P.op("act", lambda e: e.activation(gatesT[:, s * 128:(s + 1) * 128], B[2][0:32, 0:128], AF.Copy), r=[B[2]], w=[gatesT])

        def expert(ti, e, b):
            t0, n = TILES[ti]
            g_ = gm[e % 2]
            for j in range(4):
                pA, pB = B[(2 * j) % 4], B[(2 * j) % 4 + 1]
                for (ps, c0) in ((pA, j * 128), (pB, 512 + j * 128)):
                    for k in range(KC):
                        P.op("pe", lambda e_, ps=ps, c0=c0, k=k: e_.matmul(ps[:, :n], wgu[b][k // 4][:, k % 4, c0:c0 + 128], hbf[:, k, :n],
                                                                           start=(k == 0), stop=(k == KC - 1)),
                             r=[wgu[b][k // 4], hbf], w=[ps])
                if j == 0:
                    P.op("dve", lambda e_: e_.tensor_scalar(g_[:, :n], gatesT[:, :n], C.identf[0:32, e:e + 1], None, ALU.mult),
                         r=[gatesT, C.identf], w=[g_])
                    P.op("pe", lambda e_: e_.matmul(B[4][:, :n], C.ones[0:32, :], g_[:, :n], start=True, stop=True), r=[g_, C.ones], w=[B[4]])
                s_, t_ = tS[j % 2], tT[j % 2]
                P.op("act", lambda e_, s_=s_, pA=pA: e_.activation(s_[:, :n], pA[:, :n], AF.Silu), r=[pA], w=[s_])
                P.op("dve", lambda e_, s_=s_, t_=t_, pB=pB: e_.tensor_tensor(t_[:, :n], s_[:, :n], pB[:, :n], ALU.mult), r=[s_, pB], w=[t_])
                P.op("dve", lambda e_, t_=t_, j=j: e_.tensor_tensor(u[b][j][:, :n], t_[:, :n], B[4][:, :n], ALU.mult), r=[t_, B[4]], w=[u[b][j]])
            for m in range(KC):
                pO = B[5 + m % 2]
                for j in range(4):
                    P.op("pe", lambda e_, pO=pO, j=j, m=m: e_.matmul(pO[:, :n], wdn[b][j // 2][:, j % 2, m * 128:(m + 1) * 128], u[b][j][:, :n],
                                                                     start=(j == 0), stop=(j == 3)), r=[wdn[b][j // 2], u[b][j]], w=[pO])
                if e == 0:
                    P.op("act", lambda e_, pO=pO, m=m: e_.activation(buf[:, m, :n], pO[:, :n], AF.Copy), r=[pO], w=[bd[m]])
                else:
                    P.op("dve", lambda e_, pO=pO, m=m: e_.tensor_tensor(buf[:, m, :n], buf[:, m, :n], pO[:, :n], ALU.add), r=[pO, bd[m]], w=[bd[m]])

        def epilogue(ti):
            t0, n = TILES[ti]
            cs = 1 if ti == 0 else 0
            gt = gate_ap(C, 1, cs)
            for q4 in range(4):
                x_ = xs[q4 % 2]
                P.dma(x_[:, :, :n], C.XTv[:, q4 * 4:(q4 + 1) * 4, t0:t0 + n], r=[C.xdep[ti]], w=[x_])
                for kk in range(4):
                    k = q4 * 4 + kk
                    P.op("dve", lambda e_, x_=x_, kk=kk, k=k: e_.scalar_tensor_tensor(x_[:, kk, :n], buf[:, k, :n], gt[:, k:k + 1], x_[:, kk, :n],
                                                                                      ALU.mult, ALU.add), r=[bd[k], x_, C.mod], w=[x_])
                P.dma(C.XTv[:, q4 * 4:(q4 + 1) * 4, t0:t0 + n], x_[:, :, :n], r=[x_], w=[C.xdep[ti]])

        issue_w(0)
        for idx, (ti, e) in enumerate(seq):
            if e == 0:
                prologue(ti)
            if idx + 1 < len(seq):
                issue_w(idx + 1)
            expert(ti, e, idx % 2)
            if e == NE - 1:
                epilogue(ti)
        barrier(P)
        P.es = old
    barrier(P)


def final_phase(C, n_layers):
    P = C.P
    with ExitStack() as es2:
        old = P.es
        P.es = es2
        xbuf = P.sb([128, KC, 512], F32)
        tmp = [P.sb([128, 512], F32) for _ in range(2)]
        rstd = P.sb([128, 512], F32)
        for ti in range(1, 9):
            t0, n = TILES[ti]
            load_x(C, xbuf, ti)
            ps = C.psb[0]
            for k in range(KC):
                t = tmp[k % 2]
                P.op("act", lambda e, t=t, k=k: e.activation(t[:, :n], xbuf[:, k, :n], AF.Square), r=[xbuf], w=[t])
                P.op("pe", lambda e, t=t, k=k: e.matmul(ps[:, :n], C.ones[:], t[:, :n], start=(k == 0), stop=(k == KC - 1)),
                     r=[t, C.ones], w=[ps])
            P.op("act", lambda e: e.activation(rstd[:, :n], ps[:, :n], AF.Sqrt, bias=C.epsb[:, 0:1], scale=1.0 / D),
                 r=[ps, C.epsb], w=[rstd])
            P.op("dve", lambda e: e.reciprocal(rstd[:, :n], rstd[:, :n]), r=[rstd], w=[rstd])
            for k in range(KC):
                P.op("dve", lambda e, k=k: e.scalar_tensor_tensor(xbuf[:, k, :n], xbuf[:, k, :n], C.gfin_s[:, k:k + 1], rstd[:, :n],
                                                                  ALU.mult, ALU.mult), r=[xbuf, C.gfin_s, rstd], w=[xbuf])
            for h in range(2):
                P.dma(C.outTv[:, h * 8:(h + 1) * 8, t0 - NCTX:t0 - NCTX + n], xbuf[:, h * 8:(h + 1) * 8, :n], r=[xbuf], w=[C.outT])
        barrier(P)
        P.es = old


TBK = 32
STILES = [(0, 256)] + [(256 + 256 * i, 256) for i in range(16)]
TWO_PI = 6.283185307179586


def rev_last(ap):
    a = [list(x) for x in ap.ap]
    step, cnt = a[-1]
    a[-1] = [-step, cnt]
    return bass.AP(ap.tensor, ap.offset + (cnt - 1) * step, a)


def s5_phase(C, li, with_ctx):
    P = C.P
    B = C.psb
    TB = TBK
    with ExitStack() as es2:
        old = P.es
        P.es = es2
        xbuf = P.sb([128, KC, 256], F32)
        hbf = P.sb([128, KC, 256], BF16)
        tmp = [P.sb([128, 256], F32)]
        rstd = P.sb([128, 256], F32)
        BT = [P.sb([128, 64, 128], BF16) for _ in range(2)]
        CP = [P.sb([128, 64, 128], BF16) for _ in range(2)]
        COS = P.sb([128, 64, TB], F32)
        SIN = P.sb([128, 64, TB], F32)
        RT = P.sb([128, 64, TB], F32)
        G0 = P.sb([128, 2, 32, TB], F32)
        G1 = P.sb([128, 2, 32, TB], F32)
        TA = P.sb([128, 32, TB], F32)
        TBb = P.sb([128, 32, TB], F32)
        Xb = P.sb([128, 2, 32, TB], BF16)
        Y = P.sb([128, KC, 256], F32)
        ybf = P.sb([128, KC, 256], BF16)
        ring = [[P.sb([128, 4, 256], BF16) for _ in range(4)] for _ in range(2)]
        rp = [0]
        ki = P.sb([128, 1024], I32)
        tau = P.sb([128, TB], F32)
        dsk = P.sb([128, KC], F32)
        sm = [P.sb([128, 64], F32, "s5sm%d" % i) for i in range(16)]
        ar, ai, ldt, dar, dai, mag, sinv, cosv, lr, lim, zr, zi, t1, t2, den, t3 = sm
        xin = P.sb([128, 2, 64], F32)
        lamx = P.sb([128, 2, 64], F32)
        s1 = P.sb([128, 64], F32)
        s2 = P.sb([128, 64], F32)
        yfdep = [Dep() for _ in STILES]
        P.dma(tau[:], C.k_tau.t[:, :], w=[tau])
        P.dma(dsk[:], C.s5_d.t[:, :], w=[dsk])
        for c in range(2):
            for q4 in range(4):
                P.dma(CP[c][:, q4 * 16:(q4 + 1) * 16, :], C.s5_cp.t[c, :, q4 * 16:(q4 + 1) * 16, :], w=[CP[c]], q="pool")
        P.op("dve", lambda e: e.tensor_scalar(CP[1][:], CP[1][:], -1.0, None, ALU.mult), r=[CP[1]], w=[CP[1]])
        wgl = C.s5_wglu.t[0].rearrange("(k p) n -> p k n", p=128)

        def load_group(src, col0):
            buf = ring[rp[0] % 2]
            rp[0] += 1
            for q4 in range(4):
                P.dma(buf[q4][:], src[:, q4 * 4:(q4 + 1) * 4, col0:col0 + 256], w=[buf[q4]], q="pool")
            return buf

        def sincos(ang, F, osin, ocos, scratch):
            for (o, sh) in ((osin, 0.0), (ocos, 0.25)):
                P.op("dve", lambda e, sh=sh: e.tensor_scalar(scratch, ang, 1.0 / TWO_PI, sh, ALU.mult, ALU.add), r=[ang_dep[0]], w=[scr_dep[0]])
                for f0 in range(0, F, 1024):
                    f1 = min(F, f0 + 1024)
                    P.op("dve", lambda e, f0=f0, f1=f1: e.tensor_copy(ki[:, :f1 - f0], scratch[:, f0:f1]), r=[scr_dep[0]], w=[ki])
                    P.op("dve", lambda e, o=o, f0=f0, f1=f1: e.tensor_copy(o[:, f0:f1], ki[:, :f1 - f0]), r=[ki], w=[o_dep[0]])
                P.op("dve", lambda e, o=o: e.tensor_tensor(o, scratch, o, ALU.subtract), r=[scr_dep[0], o_dep[0]], w=[o_dep[0]])
                P.op("dve", lambda e, o=o: e.tensor_scalar(o, o, 0.4999995, -0.4999995, ALU.min, ALU.max), r=[o_dep[0]], w=[o_dep[0]])
                P.op("act", lambda e, o=o: e.activation(o, o, AF.Sin, scale=TWO_PI), r=[o_dep[0]], w=[o_dep[0]])

        setup = Dep()
        ang_dep = [setup]
        scr_dep = [setup]
        o_dep = [setup]
        SD = lambda fn, eng="dve": P.op(eng, fn, r=[setup], w=[setup])
        flat = lambda t: t[:].rearrange("p a b -> p (a b)")

        for d in range(2):
            P.dma(ar[:], C.s5_a.t[0, d], w=[setup])
            P.dma(ai[:], C.s5_a.t[1, d], w=[setup])
            P.dma(ldt[:], C.s5_a.t[2, d], w=[setup])
            SD(lambda e: e.activation(ldt[:], ldt[:], AF.Exp), "act")
            SD(lambda e: e.tensor_tensor(dar[:], ldt[:], ar[:], ALU.mult))
            SD(lambda e: e.tensor_tensor(dai[:], ldt[:], ai[:], ALU.mult))
            SD(lambda e: e.activation(mag[:], dar[:], AF.Exp), "act")
            sincos(dai[:], 64, sinv[:], cosv[:], t3[:])
            SD(lambda e: e.tensor_tensor(lr[:], mag[:], cosv[:], ALU.mult))
            SD(lambda e: e.tensor_tensor(lim[:], mag[:], sinv[:], ALU.mult))
            SD(lambda e: e.tensor_scalar(t1[:], lr[:], -1.0, None, ALU.add))
            SD(lambda e: e.tensor_tensor(den[:], ar[:], ar[:], ALU.mult))
            SD(lambda e: e.tensor_tensor(t2[:], ai[:], ai[:], ALU.mult))
            SD(lambda e: e.tensor_tensor(den[:], den[:], t2[:], ALU.add))
            SD(lambda e: e.reciprocal(den[:], den[:]))
            SD(lambda e: e.tensor_tensor(zr[:], t1[:], ar[:], ALU.mult))
            SD(lambda e: e.tensor_tensor(t2[:], lim[:], ai[:], ALU.mult))
            SD(lambda e: e.tensor_tensor(zr[:], zr[:], t2[:], ALU.add))
            SD(lambda e: e.tensor_tensor(zr[:], zr[:], den[:], ALU.mult))
            SD(lambda e: e.tensor_tensor(zi[:], lim[:], ar[:], ALU.mult))
            SD(lambda e: e.tensor_tensor(t2[:], t1[:], ai[:], ALU.mult))
            SD(lambda e: e.tensor_tensor(zi[:], zi[:], t2[:], ALU.subtract))
            SD(lambda e: e.tensor_tensor(zi[:], zi[:], den[:], ALU.mult))
            SD(lambda e: e.tensor_tensor(TA[:] if False else COS[:], dai[:].unsqueeze(2).to_broadcast([128, 64, TB]),
                                         tau[:].unsqueeze(1).to_broadcast([128, 64, TB]), ALU.mult))
            SD(lambda e: e.tensor_copy(RT[:], COS[:]))
            sincos(flat(RT), 64 * TB, flat(SIN), flat(COS), flat(G0)[:, 0:64 * TB] if False else G0[:].rearrange("p a b c -> p (a b c)"))
            SD(lambda e: e.tensor_copy(RT[:], mag[:].unsqueeze(2).to_broadcast([128, 64, TB])))
            SD(lambda e: e.memset(RT[:, :, 0:1], 0.0))
            for jc in range(4):
                js = slice(jc * 16, (jc + 1) * 16)
                bre = G0[:].rearrange("p a b c -> p (a b c)").rearrange("p (j x) -> p j x", x=128)
                bim = G1[:].rearrange("p a b c -> p (a b c)").rearrange("p (j x) -> p j x", x=128)
                u1 = TA[:].rearrange("p a b -> p (a b)")
                P.dma(bre, C.s5_bp.t[0, :, js, :], w=[setup])
                P.dma(bim, C.s5_bp.t[1, :, js, :], w=[setup])
                o1 = Y[:].rearrange("p a b -> p (a b)")[:, 0:2048].rearrange("p (j x) -> p j x", x=128)
                o2 = Y[:].rearrange("p a b -> p (a b)")[:, 2048:4096].rearrange("p (j x) -> p j x", x=128)
                w1 = xbuf[:].rearrange("p a b -> p (a b)")[:, 0:2048].rearrange("p (j x) -> p j x", x=128)
                zrb = zr[:, js].unsqueeze(2).to_broadcast([128, 16, 128])
                zib = zi[:, js].unsqueeze(2).to_broadcast([128, 16, 128])
                SD(lambda e, zrb=zrb: e.tensor_tensor(o1, bre, zrb, ALU.mult))
                SD(lambda e, zib=zib: e.tensor_tensor(w1, bim, zib, ALU.mult))
                SD(lambda e: e.tensor_tensor(o1, o1, w1, ALU.subtract))
                SD(lambda e, zib=zib: e.tensor_tensor(o2, bre, zib, ALU.mult))
                SD(lambda e, zrb=zrb: e.tensor_tensor(w1, bim, zrb, ALU.mult))
                SD(lambda e: e.tensor_tensor(o2, o2, w1, ALU.add))
                for c, oo in ((0, o1), (1, o2)):
                    for j4 in range(4):
                        for jj in range(4):
                            jl = j4 * 4 + jj
                            P.op("pe", lambda e, oo=oo, jl=jl, jj=jj: e.transpose(B[0][:, jj * 128:(jj + 1) * 128], oo[:, jl, :], C.identf[:]),
                                 r=[setup, C.identf], w=[B[0]])
                        P.op("act", lambda e, c=c, jc=jc, j4=j4: e.activation(BT[c][:, jc * 16 + j4 * 4:jc * 16 + j4 * 4 + 4, :],
                                                                             B[0][:, :].rearrange("p (j x) -> p j x", x=128), AF.Copy),
                             r=[B[0]], w=[BT[c]])
            barrier(P)
            P.op("dve", lambda e: e.memset(xin[:], 0.0), w=[xin])
            P.op("dve", lambda e: e.memset(lamx[:], 0.0), w=[lamx])
            order = list(range(17)) if d == 0 else [0] + list(range(16, 0, -1))
            for si in order:
                t0, n = STILES[si]
                pti = 0 if si == 0 else 1 + (si - 1) // 2
                cs_ = 1 if si == 0 else 0
                for hx in range(2):
                    P.dma(xbuf[:, hx * 8:(hx + 1) * 8, :n], C.XTv[:, hx * 8:(hx + 1) * 8, t0:t0 + n], r=[C.xdep[pti]], w=[xbuf])
                norm_mod(C, xbuf, n, cs_, hbf, tmp, rstd, psbank=0)
                nblk = n // TB
                blks = list(range(nblk)) if d == 0 else list(range(nblk - 1, -1, -1))
                for bi in blks:
                    c0 = bi * TB
                    for hf in range(2):
                        s5_block(C, d, hf, c0, hbf, BT, CP, COS, SIN, RT, G0, G1, TA, TBb, Xb, Y, xin, lamx, lr, lim, s1, s2)
                if d == 0:
                    for hx in range(2):
                        P.dma(C.OFT[:, t0:t0 + n].rearrange("(k p) t -> p k t", p=128)[:, hx * 8:(hx + 1) * 8, :], Y[:, hx * 8:(hx + 1) * 8, :n],
                              r=[Y], w=[yfdep[si]])
                else:
                    for hx in range(2):
                        P.dma(xbuf[:, hx * 8:(hx + 1) * 8, :n], C.OFT[:, t0:t0 + n].rearrange("(k p) t -> p k t", p=128)[:, hx * 8:(hx + 1) * 8, :],
                              r=[yfdep[si]], w=[xbuf])
                    for k in range(KC):
                        P.op("dve", lambda e, k=k, n=n: e.tensor_tensor(Y[:, k, :n], Y[:, k, :n], xbuf[:, k, :n], ALU.add), r=[Y, xbuf], w=[Y])
                        P.op("dve", lambda e, k=k, n=n: e.scalar_tensor_tensor(ybf[:, k, :n], hbf[:, k, :n], dsk[:, k:k + 1], Y[:, k, :n], ALU.mult, ALU.add),
                             r=[Y, hbf, dsk], w=[ybf])
                    for hx in range(2):
                        P.dma(xbuf[:, hx * 8:(hx + 1) * 8, :n], C.XTv[:, hx * 8:(hx + 1) * 8, t0:t0 + n], r=[C.xdep[pti]], w=[xbuf])
                    gt = gate_ap(C, 0, cs_)
                    for mg in range(8):
                        Wa = load_group(wgl, mg * 256)
                        Wg = load_group(wgl, 2048 + mg * 256)
                        for mm in range(2):
                            m = mg * 2 + mm
                            for (Wx, ps) in ((Wa, B[5]), (Wg, B[6])):
                                for k in range(KC):
                                    P.op("pe", lambda e, Wx=Wx, ps=ps, k=k, mm=mm, n=n: e.matmul(ps[:, :n], Wx[k // 4][:, k % 4, mm * 128:(mm + 1) * 128], ybf[:, k, :n],
                                                                                              start=(k == 0), stop=(k == KC - 1)), r=[Wx[k // 4], ybf], w=[ps])
                            P.op("act", lambda e, n=n: e.activation(tmp[0][:, :n], B[6][:, :n], AF.Sigmoid), r=[B[6]], w=[tmp[0]])
                            P.op("dve", lambda e, n=n: e.tensor_tensor(tmp[0][:, :n], tmp[0][:, :n], B[5][:, :n], ALU.mult), r=[tmp[0], B[5]], w=[tmp[0]])
                            P.op("dve", lambda e, m=m, gt=gt, n=n: e.scalar_tensor_tensor(xbuf[:, m, :n], tmp[0][:, :n], gt[:, m:m + 1], xbuf[:, m, :n], ALU.mult, ALU.add),
                                 r=[tmp[0], xbuf, C.mod], w=[xbuf])
                    for hx in range(2):
                        P.dma(C.XTv[:, hx * 8:(hx + 1) * 8, t0:t0 + n], xbuf[:, hx * 8:(hx + 1) * 8, :n], r=[xbuf], w=[C.xdep[pti]])
            barrier(P)
        barrier(P)
        P.es = old
    barrier(P)


def s5_block(C, d, hf, c0, hbf, BT, CP, COS, SIN, RT, G0, G1, TA, TBb, Xb, Y, xin, lamx, lr, lim, s1, s2):
    P = C.P
    B = C.psb
    TB = TBK
    j0 = hf * 32
    js = slice(j0, j0 + 32)
    for c in range(2):
        for jl in range(32):
            j = j0 + jl
            bank = B[c * 2 + jl // 16]
            P.op("pe", lambda e, c=c, j=j, jl=jl, bank=bank: e.matmul(bank[:, (jl % 16) * TB:(jl % 16 + 1) * TB], BT[c][:, j, :], hbf[:, j // 4, c0:c0 + TB],
                                                                      start=True, stop=True), r=[BT[c], hbf], w=[bank])
        for hb in range(2):
            src = B[c * 2 + hb][:, :].rearrange("p (j t) -> p j t", t=TB)
            if d == 1:
                src = rev_last(src)
            P.op("act", lambda e, c=c, hb=hb, src=src: e.activation(G0[:, c, hb * 16:(hb + 1) * 16, :], src, AF.Copy), r=[B[c * 2 + hb]], w=[G0])
    cs, sn, rt = COS[:, js, :], SIN[:, js, :], RT[:, js, :]
    bur, bui = G0[:, 0], G0[:, 1]
    vr, vi = G1[:, 0], G1[:, 1]
    D = lambda fn, r, w: P.op("dve", fn, r=r, w=w)
    D(lambda e: e.tensor_tensor(TA[:], bur, cs, ALU.mult), [G0, COS], [TA])
    D(lambda e: e.tensor_tensor(TBb[:], bui, sn, ALU.mult), [G0, SIN], [TBb])
    D(lambda e: e.tensor_tensor(vr, TA[:], TBb[:], ALU.add), [TA, TBb], [G1])
    D(lambda e: e.tensor_tensor(TA[:], bui, cs, ALU.mult), [G0, COS], [TA])
    D(lambda e: e.tensor_tensor(TBb[:], bur, sn, ALU.mult), [G0, SIN], [TBb])
    D(lambda e: e.tensor_tensor(vi, TA[:], TBb[:], ALU.subtract), [TA, TBb], [G1])
    D(lambda e: e.tensor_tensor(G1[:, :, :, 0], G1[:, :, :, 0], lamx[:, :, js], ALU.add), [G1, lamx], [G1])
    fl = lambda a: a.rearrange("p j t -> p (j t)")
    D(lambda e: e.tensor_tensor_scan(fl(bur), fl(rt), fl(vr), 0.0, ALU.mult, ALU.add), [RT, G1], [G0])
    D(lambda e: e.tensor_tensor_scan(fl(bui), fl(rt), fl(vi), 0.0, ALU.mult, ALU.add), [RT, G1], [G0])
    D(lambda e: e.tensor_tensor(TA[:], bur, cs, ALU.mult), [G0, COS], [TA])
    D(lambda e: e.tensor_tensor(TBb[:], bui, sn, ALU.mult), [G0, SIN], [TBb])
    D(lambda e: e.tensor_tensor(vr, TA[:], TBb[:], ALU.subtract), [TA, TBb], [G1])
    D(lambda e: e.tensor_tensor(TA[:], bur, sn, ALU.mult), [G0, SIN], [TA])
    D(lambda e: e.tensor_tensor(TBb[:], bui, cs, ALU.mult), [G0, COS], [TBb])
    D(lambda e: e.tensor_tensor(vi, TA[:], TBb[:], ALU.add), [TA, TBb], [G1])
    P.op("act", lambda e: e.activation(Xb[:, 0], vr, AF.Copy), r=[G1], w=[Xb])
    P.op("act", lambda e: e.activation(Xb[:, 1], vi, AF.Copy), r=[G1], w=[Xb])
    D(lambda e: e.tensor_copy(xin[:, :, js], G1[:, :, :, TB - 1]), [G1], [xin])
    D(lambda e: e.tensor_tensor(s1[:, js], lr[:, js], xin[:, 0, js], ALU.mult), [xin, lr], [s1])
    D(lambda e: e.tensor_tensor(s2[:, js], lim[:, js], xin[:, 1, js], ALU.mult), [xin, lim], [s2])
    D(lambda e: e.tensor_tensor(lamx[:, 0, js], s1[:, js], s2[:, js], ALU.subtract), [s1, s2], [lamx])
    D(lambda e: e.tensor_tensor(s1[:, js], lr[:, js], xin[:, 1, js], ALU.mult), [xin, lr], [s1])
    D(lambda e: e.tensor_tensor(s2[:, js], lim[:, js], xin[:, 0, js], ALU.mult), [xin, lim], [s2])
    D(lambda e: e.tensor_tensor(lamx[:, 1, js], s1[:, js], s2[:, js], ALU.add), [s1, s2], [lamx])
    for kk in range(8):
        kc = hf * 8 + kk
        for jj in range(4):
            for c in range(2):
                jl = kk * 4 + jj
                P.op("pe", lambda e, kk=kk, jl=jl, c=c: e.matmul(B[4][:, kk * TB:(kk + 1) * TB], CP[c][:, j0 + jl, :], Xb[:, c, jl, :],
                                                                 start=(jj == 0 and c == 0), stop=(jj == 3 and c == 1)), r=[CP[c], Xb], w=[B[4]]) \
                    if False else P.op("pe", (lambda kk, jl, c, jj: (lambda e: e.matmul(B[4][:, kk * TB:(kk + 1) * TB], CP[c][:, j0 + jl, :], Xb[:, c, jl, :],
                                                                                start=(jj == 0 and c == 0), stop=(jj == 3 and c == 1))))(kk, jl, c, jj),
                                       r=[CP[c], Xb], w=[B[4]])
    src = B[4][:, 0:8 * TB].rearrange("p (k t) -> p k t", t=TB)
    if d == 1:
        src = rev_last(src)
    P.op("act", lambda e: e.activation(Y[:, hf * 8:(hf + 1) * 8, c0:c0 + TB], src, AF.Copy), r=[B[4]], w=[Y])


def attn_phase(C, li, with_ctx):
    P = C.P
    B = C.psb
    SC = 64.0 ** -0.5
    NEG = -1.0e4
    with ExitStack() as es2:
        old = P.es
        P.es = es2
        xbuf = P.sb([128, KC, 512], F32)
        hbf = P.sb([128, KC, 512], BF16)
        tmp = [P.sb([128, 512], F32)]
        rstd = P.sb([128, 512], F32)
        kT = P.sb([128, 4, NT], BF16)
        vall = P.sb([128, 34, 512], BF16)
        ring = [[P.sb([128, 4, 512], BF16) for _ in range(4)] for _ in range(2)]
        rp = [0]
        rope = P.sb([128, 2, 2, 16], F32)
        ktok = P.sb([128, 512], F32)
        krot = P.sb([128, 512], BF16)
        rt1 = P.sb([128, 512], F32)
        qtok = P.sb([128, 4, 2048], BF16)
        qT = P.sb([128, 16, 128], BF16)
        ssb = P.sb([128, 640], F32)
        pb = P.sb([128, 640], BF16)
        pT = P.sb([128, 640], BF16)
        otok = P.sb([128, 2048], BF16)
        oT = P.sb([128, KC, 512], BF16)
        sm = P.sb([128, 8], F32)
        sinkb = P.sb([128, 32], F32)
        maskLR = P.sb([128, 2, 128], F32)
        P.dma(sinkb[:], C.sink.t[0:1, :].partition_broadcast(128), w=[sinkb])
        P.dma(maskLR[:], C.k_amask.t[:, :].rearrange("p (a b) -> p a b", a=2), w=[maskLR])
        wv = C.w_qkv.t[0].rearrange("(k p) n -> p k n", p=128)
        wov = C.w_o.t[0].rearrange("(k p) n -> p k n", p=128)

        def load_group(src, col0):
            buf = ring[rp[0] % 2]
            rp[0] += 1
            for q4 in range(4):
                P.dma(buf[q4][:], src[:, q4 * 4:(q4 + 1) * 4, col0:col0 + 512], w=[buf[q4]], q="pool")
            return buf

        def load_group_perm(src, base, pairs):
            buf = ring[rp[0] % 2]
            rp[0] += 1
            for i, (hA, hB) in enumerate(pairs):
                for q4 in range(4):
                    for j_, hX in enumerate((hA, hB)):
                        P.dma(buf[q4][:, :, i * 128 + j_ * 64:i * 128 + (j_ + 1) * 64],
                              src[:, q4 * 4:(q4 + 1) * 4, base + hX * 64:base + (hX + 1) * 64], w=[buf[q4]], q="pool")
            return buf

        def do_rope(src, dst, nh, blk):
            P.dma(rope[:], C.k_rope.t[blk * 128:(blk + 1) * 128, :].rearrange("p (a b c) -> p a b c", a=2, b=2), w=[rope])
            v = lambda t: t[:, :nh * 64].rearrange("p (h a b c) -> p (h a) b c", a=2, b=2, c=16)
            cs = lambda i: rope[:, i, :, :].unsqueeze(1).to_broadcast([128, nh, 2, 16]).rearrange("p h a c -> p (h a) c") \
                if False else None
            x1 = v(src)[:, :, 0, :]
            x2 = v(src)[:, :, 1, :]
            d1 = v(dst)[:, :, 0, :]
            d2 = v(dst)[:, :, 1, :]
            t1 = v(rt1)[:, :, 0, :]
            t2 = v(rt1)[:, :, 1, :]
            Cc = rope[:, 0, :, :].unsqueeze(1).to_broadcast([128, nh, 2, 16])
            Ss = rope[:, 1, :, :].unsqueeze(1).to_broadcast([128, nh, 2, 16])
            r4 = lambda a: a.rearrange("p (h a) c -> p h a c", a=2)
            P.op("dve", lambda e: e.tensor_tensor(r4(t1), r4(x1), Cc, ALU.mult), r=[src, rope], w=[rt1])
            P.op("dve", lambda e: e.tensor_tensor(r4(t2), r4(x2), Ss, ALU.mult), r=[src, rope], w=[rt1])
            P.op("dve", lambda e: e.tensor_tensor(d1, t1, t2, ALU.subtract), r=[rt1], w=[dst])
            P.op("dve", lambda e: e.tensor_tensor(r4(t1), r4(x1), Ss, ALU.mult), r=[src, rope, dst], w=[rt1])
            P.op("dve", lambda e: e.tensor_tensor(r4(t2), r4(x2), Cc, ALU.mult), r=[src, rope], w=[rt1])
            P.op("dve", lambda e: e.tensor_tensor(d2, t1, t2, ALU.add), r=[rt1], w=[dst])

        for ti in range(9):
            t0, n = TILES[ti]
            load_x(C, xbuf, ti)
            norm_mod(C, xbuf, n, 1 if ti == 0 else 0, hbf, tmp, rstd, psbank=0)
            Wk = load_group_perm(wv, 2048, [(g_, g_ + 4) for g_ in range(4)])
            Wv = load_group(wv, 2560)
            for bl in range(n // 128):
                gb = t0 // 128 + bl
                for (Wp, ps) in ((Wk, B[1]), (Wv, B[2])):
                    for k in range(KC):
                        P.op("pe", lambda e, Wp=Wp, ps=ps, k=k, bl=bl: e.matmul(ps[:, :], hbf[:, k, bl * 128:(bl + 1) * 128], Wp[k // 4][:, k % 4, :],
                                                                             start=(k == 0), stop=(k == KC - 1)), r=[hbf, Wp[k // 4]], w=[ps])
                P.op("act", lambda e, gb=gb: e.activation(vall[:, gb, :], B[2][:, :], AF.Copy), r=[B[2]], w=[vall])
                if ti == 0:
                    P.op("act", lambda e: e.activation(krot[:, 0:512], B[1][:, :], AF.Copy), r=[B[1]], w=[krot])
                else:
                    P.op("act", lambda e: e.activation(ktok[:, 0:512], B[1][:, :], AF.Copy), r=[B[1]], w=[ktok])
                    do_rope(ktok, krot, 8, gb - 2)
                for g in range(4):
                    P.op("pe", lambda e, g=g: e.transpose(C.psbf[:, g * 128:(g + 1) * 128],
                                                          krot[:, g * 128:(g + 1) * 128], C.identb[:]),
                         r=[krot, C.identb], w=[C.psbf])
                P.op("act", lambda e, gb=gb: e.activation(kT[:, :, gb * 128:(gb + 1) * 128], C.psbf[:, 0:512].rearrange("p (g t) -> p g t", g=4), AF.Copy),
                     r=[C.psbf], w=[kT])
        for ti in range(9):
            t0, n = TILES[ti]
            cs_ = 1 if ti == 0 else 0
            load_x(C, xbuf, ti)
            norm_mod(C, xbuf, n, cs_, hbf, tmp, rstd, psbank=0)
            for qg in range(4):
                Wq = load_group_perm(wv, 0, [(4 * qg + i_, 16 + 4 * qg + i_) for i_ in range(4)])
                for bl in range(n // 128):
                    ps = B[qg % 2]
                    for k in range(KC):
                        P.op("pe", lambda e, ps=ps, k=k, bl=bl, Wq=Wq: e.matmul(ps[:, :], hbf[:, k, bl * 128:(bl + 1) * 128], Wq[k // 4][:, k % 4, :],
                                                                         start=(k == 0), stop=(k == KC - 1)), r=[hbf, Wq[k // 4]], w=[ps])
                    if ti == 0:
                        P.op("act", lambda e, ps=ps, bl=bl, qg=qg: e.activation(qtok[:, bl, qg * 512:(qg + 1) * 512], ps[:, :], AF.Copy), r=[ps], w=[qtok])
                    else:
                        P.op("act", lambda e, ps=ps: e.activation(ktok[:, 0:512], ps[:, :], AF.Copy), r=[ps], w=[ktok])
                        do_rope(ktok, krot, 8, (t0 - NCTX) // 128 + bl)
                        P.op("pool", lambda e, bl=bl, qg=qg: e.tensor_copy(qtok[:, bl, qg * 512:(qg + 1) * 512], krot[:, 0:512]), r=[krot], w=[qtok])
            for bl in range(n // 128):
                gb = t0 // 128 + bl
                nb = gb - 2
                for hg in range(2):
                    for hh in range(8):
                        h = hg * 8 + hh
                        P.op("pe", lambda e, h=h, hh=hh, bl=bl: e.transpose(C.psbf[:, hh * 128:(hh + 1) * 128],
                                                                            qtok[:, bl, h * 128:(h + 1) * 128], C.identb[:]),
                             r=[qtok, C.identb], w=[C.psbf])
                    P.op("act", lambda e, hg=hg: e.activation(qT[:, hg * 8:(hg + 1) * 8, :], C.psbf[:, 0:1024].rearrange("p (g t) -> p g t", g=8), AF.Copy),
                         r=[C.psbf], w=[qT])
                if ti == 0:
                    kbs = [(0, None), (1, None)]
                else:
                    kbs = [(0, None), (1, None)]
                    if nb > 0:
                        kbs.append((gb - 1, 0))
                    kbs.append((gb, None))
                    if nb < 31:
                        kbs.append((gb + 1, 1))
                nk = len(kbs) * 128
                for h in range(32):
                    g = h // 4
                    bankA, bankB = B[3], B[4]
                    for i, (kb, mk) in enumerate(kbs):
                        dst = bankA[:, i * 128:(i + 1) * 128] if i < 4 else bankB[:, 0:128]
                        bk = bankA if i < 4 else bankB
                        po = 0 if h < 16 else 64
                        P.op("pe", lambda e, dst=dst, h=h, g=g, kb=kb, po=po: e.matmul(dst, qT[po:po + 64, h % 16, :], kT[po:po + 64, g % 4, kb * 128:(kb + 1) * 128],
                                                                                   start=True, stop=True), r=[qT, kT], w=[bk])
                    for i, (kb, mk) in enumerate(kbs):
                        src = bankA[:, i * 128:(i + 1) * 128] if i < 4 else bankB[:, 0:128]
                        bk = bankA if i < 4 else bankB
                        if mk is None:
                            P.op("act", lambda e, src=src, i=i: e.activation(ssb[:, i * 128:(i + 1) * 128], src, AF.Copy), r=[bk], w=[ssb])
                        else:
                            P.op("dve", lambda e, src=src, i=i, mk=mk: e.tensor_tensor(ssb[:, i * 128:(i + 1) * 128], src, maskLR[:, mk, :], ALU.add),
                                 r=[bk, maskLR], w=[ssb])
                    P.op("dve", lambda e, nk=nk: e.reduce_max(sm[:, 0:1], ssb[:, :nk], AX.X), r=[ssb], w=[sm])
                    P.op("dve", lambda e, h=h: e.tensor_scalar(sm[:, 0:1], sm[:, 0:1], SC, sinkb[:, h:h + 1], ALU.mult, ALU.max), r=[sm, sinkb], w=[sm])
                    P.op("dve", lambda e: e.tensor_scalar(sm[:, 1:2], sm[:, 0:1], -1.0, None, ALU.mult), r=[sm], w=[sm])
                    P.op("dve", lambda e: e.memset(sm[:, 2:3], 0.0), r=[sm], w=[sm])
                    P.op("act", lambda e, nk=nk: e.activation(pb[:, :nk], ssb[:, :nk], AF.Exp, bias=sm[:, 1:2], scale=SC, accum_out=sm[:, 2:3]), r=[ssb, sm], w=[pb, sm])
                    P.op("act", lambda e, h=h: e.activation(sm[:, 3:4], sm[:, 0:1], AF.Exp, bias=sinkb[:, h:h + 1], scale=-1.0), r=[sm, sinkb], w=[sm])
                    P.op("dve", lambda e: e.tensor_tensor(sm[:, 4:5], sm[:, 2:3], sm[:, 3:4], ALU.add), r=[sm], w=[sm])
                    P.op("dve", lambda e: e.reciprocal(sm[:, 5:6], sm[:, 4:5]), r=[sm], w=[sm])
                    for i in range(len(kbs)):
                        P.op("pe", lambda e, i=i: e.transpose(C.psbf[:, i * 128:(i + 1) * 128], pb[:, i * 128:(i + 1) * 128], C.identb[:]),
                             r=[pb, C.identb], w=[C.psbf])
                    P.op("act", lambda e, nk=nk: e.activation(pT[:, :nk], C.psbf[:, :nk], AF.Copy), r=[C.psbf], w=[pT])
                    for i, (kb, mk) in enumerate(kbs):
                        P.op("pe", lambda e, i=i, kb=kb, g=g, nkb=len(kbs): e.matmul(B[5][:, 0:64], pT[:, i * 128:(i + 1) * 128], vall[:, kb, g * 64:(g + 1) * 64],
                                                                       start=(i == 0), stop=(i == nkb - 1)), r=[pT, vall], w=[B[5]])
                    P.op("dve", lambda e, h=h: e.tensor_scalar(otok[:, h * 64:(h + 1) * 64], B[5][:, 0:64], sm[:, 5:6], None, ALU.mult), r=[B[5], sm], w=[otok])
                    if C.dbg and ((ti == 0 and bl == 0) or (ti == 1 and bl == 1)) and h in (0, 17):
                        x_ = "_t%d_h%d" % (ti, h)
                        tap(C, "ssb" + x_, ssb[:, :], [128, 640], [ssb])
                        tap(C, "sm" + x_, sm[:, :], [128, 8], [sm])
                        tap(C, "pb" + x_, pb[:, :], [128, 640], [pb], BF16)
                        tap(C, "pT" + x_, pT[:, :], [128, 640], [pT], BF16)
                        tap(C, "qT" + x_, qT[:, :, :], [128, 16, 128], [qT], BF16)
                        tap(C, "qtok" + x_, qtok[:, 0, :], [128, 2048], [qtok], BF16)
                        tap(C, "hbf" + x_, hbf[:, :, 0:128], [128, 16, 128], [hbf], BF16)
                for kc0 in range(0, KC, 8):
                    for kk in range(8):
                        k = kc0 + kk
                        P.op("pe", lambda e, k=k, kk=kk: e.transpose(C.psbf[:, kk * 128:(kk + 1) * 128], otok[:, k * 128:(k + 1) * 128], C.identb[:]),
                             r=[otok, C.identb], w=[C.psbf])
                    P.op("act", lambda e, kc0=kc0, bl=bl: e.activation(oT[:, kc0:kc0 + 8, bl * 128:(bl + 1) * 128], C.psbf[:, 0:1024].rearrange("p (k t) -> p k t", k=8), AF.Copy),
                         r=[C.psbf], w=[oT])
            if C.dbg and ti in (0, 1):
                tap(C, "otok_t%d" % ti, otok[:, :], [128, 2048], [otok], BF16)
                tap(C, "oT_t%d" % ti, oT[:, :, :], [128, 16, 512], [oT], BF16)
                if ti == 0:
                    tap(C, "kT", kT[:, :, 0:1024], [128, 4, 1024], [kT], BF16)
                    tap(C, "kTfull", kT[:, :, :], [128, 4, NT], [kT], BF16)
                    tap(C, "vall", vall[:, 0:8, :], [128, 8, 512], [vall], BF16)
            for mg in range(4):
                Wo = load_group(wov, mg * 512)
                for mm in range(4):
                    m = mg * 4 + mm
                    ps = B[m % 2]
                    for k in range(KC):
                        P.op("pe", lambda e, ps=ps, Wo=Wo, k=k, mm=mm, n=n: e.matmul(ps[:, :n], Wo[k // 4][:, k % 4, mm * 128:(mm + 1) * 128], oT[:, k, :n],
                                                                                start=(k == 0), stop=(k == KC - 1)), r=[Wo[k // 4], oT], w=[ps])
                    gt = gate_ap(C, 0, cs_)
                    P.op("dve", lambda e, ps=ps, m=m, gt=gt, n=n: e.scalar_tensor_tensor(xbuf[:, m, :n], ps[:, :n], gt[:, m:m + 1], xbuf[:, m, :n], ALU.mult, ALU.add),
                         r=[ps, xbuf, C.mod], w=[xbuf])
            store_x(C, xbuf, ti)
        barrier(P)
        P.es = old
    barrier(P)


_NC_CACHE = {}


def _consts():
    k = {}
    sel = np.zeros((32, NE * 128), np.float32)
    for e in range(NE):
        sel[e, e * 128:(e + 1) * 128] = 1.0
    k["k_sel"] = sel
    s = np.arange(CH)[:, None]
    t = np.arange(CH)[None, :]
    k["k_mask"] = np.concatenate([(s <= t), (s >= t)], axis=1).astype(np.float32)
    tt = np.arange(LSEQ)
    invf = (10000.0 ** (-np.arange(16, dtype=np.float32) / 16)).astype(np.float32)
    ang_r = (tt // 64).astype(np.float32)[:, None] * invf
    ang_c = (tt % 64).astype(np.float32)[:, None] * invf
    rope = np.stack([np.stack([np.cos(ang_r), np.cos(ang_c)], 1), np.stack([np.sin(ang_r), np.sin(ang_c)], 1)], 1)
    k["k_rope"] = np.ascontiguousarray(rope.reshape(LSEQ, 64).astype(np.float32))
    i_ = np.arange(128)[:, None]
    j_ = np.arange(128)[None, :]
    k["k_amask"] = np.concatenate([np.where(j_ >= i_, 0.0, -1.0e4), np.where(j_ <= i_, 0.0, -1.0e4)], axis=1).astype(np.float32)
    k["k_tau"] = np.ascontiguousarray(np.broadcast_to(np.arange(TBK, dtype=np.float32)[None, :], (128, TBK)))
    k["k_cum"] = np.zeros((64, 128), np.float32)
    k["k_selb"] = np.zeros((64, 6), np.float32)
    return k


def _fm(v):
    v = np.asarray(v, np.float32)
    lead = v.shape[:-1]
    return np.ascontiguousarray(np.moveaxis(v.reshape(lead + (KC, 128)), -1, 0))


def prep_s5(inp):
    f = np.float32
    m = {}
    def st(v):
        return v.reshape(2, 64, 2, 64).transpose(0, 2, 3, 1).reshape(2, 128, 64)
    a_re = st(inp["s5_a_re"][0]); a_im = st(inp["s5_a_im"][0])
    ldt = st(np.broadcast_to(inp["s5_log_dt"][0][:, :, None], (2, 128, 64)))
    m["s5_a"] = np.ascontiguousarray(np.stack([a_re, a_im, ldt], 0).astype(f))
    bp = np.zeros((2, 128, 64, 128), f)
    cp = np.zeros((2, 128, 64, 128), f)
    for ci, (bsrc, csrc) in enumerate(((inp["s5_b_re"][0], inp["s5_c_re"][0]), (inp["s5_b_im"][0], inp["s5_c_im"][0]))):
        for g2 in range(2):
            for j in range(64):
                g = 2 * j + g2
                col = 32 * (j % 4) + 16 * g2
                bp[ci, g2 * 64:(g2 + 1) * 64, j, col:col + 16] = bsrc[g]
                cp[ci, g2 * 64:(g2 + 1) * 64, j, col:col + 16] = csrc[g].T
    m["s5_bp"] = bp
    m["s5_cp"] = cp
    m["s5_d"] = np.ascontiguousarray(inp["s5_d"][0].reshape(KC, 128).T.astype(f))
    m["s5_w_glu"] = inp["s5_w_glu"]
    return m


def prep_shared(inp):
    f = np.float32
    m = {}
    m["ada_w"] = inp["ada_w"]
    m["ada_b"] = np.ascontiguousarray(inp["ada_b"].reshape(4, 96, 128).transpose(0, 2, 1).astype(f))
    m["gmix"] = np.ascontiguousarray(inp["norm_mix_g"].reshape(4, KC, 128).transpose(2, 0, 1).reshape(128, 4 * KC).astype(f))
    m["gffn"] = np.ascontiguousarray(inp["norm_ffn_g"].reshape(4, KC, 128).transpose(2, 0, 1).reshape(128, 4 * KC).astype(f))
    m["gfin"] = np.ascontiguousarray(inp["final_norm_g"].reshape(KC, 128).T.astype(f))
    m["hgrn_w_in"] = inp["hgrn_w_in"]
    m["hgrn_w_out"] = inp["hgrn_w_out"]
    m["hgrn_gn"] = inp["hgrn_gnorm_g"]
    m["hgrn_lb"] = np.ascontiguousarray(inp["hgrn_lb_logits"].reshape(4, 16, 128).transpose(2, 0, 1).astype(f))
    m.update(prep_s5(inp))
    m["attn_w_qkv"] = inp["attn_w_qkv"]
    m["attn_w_o"] = inp["attn_w_o"]
    m["attn_sink"] = inp["attn_sink"]
    m["wr"] = np.ascontiguousarray(np.concatenate([inp["moe_w_group"], inp["moe_w_expert"]], axis=-1).astype(f))
    m["br"] = np.ascontiguousarray(np.concatenate([inp["moe_b_group"], inp["moe_b_expert"]], axis=-1).astype(f))
    m["moe_w_gate_up"] = inp["moe_w_gate_up"]
    m["moe_w_down"] = inp["moe_w_down"]
    m.update(_consts())
    return m


def prep_core(inp, b):
    f = np.float32
    m = {}
    m["xT0"] = np.ascontiguousarray(np.concatenate([inp["ctx"][b].T, inp["x"][b].T], axis=1).astype(f))
    c2 = np.stack([inp["c"][b], inp["c_ctx"]], axis=-1)
    m["c2"] = np.ascontiguousarray(c2.reshape(KC, 128, 2).transpose(1, 0, 2).astype(f))
    return m


def prep_inputs(inp, b):
    m = prep_shared(inp)
    m.update(prep_core(inp, b))
    return m


def kernel(**inp):
    inp = {k: np.asarray(v) for k, v in inp.items()}
    if "nc" not in _NC_CACHE:
        _NC_CACHE["nc"] = build(4)
    nc = _NC_CACHE["nc"]
    shared = prep_shared(inp)
    in_maps = []
    for b in range(8):
        m = dict(shared)
        m.update(prep_core(inp, b))
        in_maps.append(m)
    res = run_bass_kernel_spmd(nc, in_maps, core_ids=list(range(8)))
    out = np.stack([np.ascontiguousarray(r["outT"].T) for r in res.results], axis=0)
    return out.astype(np.float32)
```
